# Optimizing a Trainium2 kernel written in Bass

```python
import numpy as np
import jax
import jax.numpy as jnp
from jax import lax

D_MODEL = 1024
BATCH = 8
SEQ = 2048
DEPTH = 1

GRID_W = 64
CTX_LEN = 256
ADALN_CHUNKS = 6
NORM_EPS = 1e-6

GLA_HEADS = 4
GLA_DV = D_MODEL // GLA_HEADS
GLA_DK = GLA_DV // 2
GLA_KEY_W = GLA_HEADS * GLA_DK
GLA_VAL_W = GLA_HEADS * GLA_DV
GLA_LORA = 16
GLA_GATE_NORM = 16.0
GLA_CHUNK = 64

RWKV_N = 64
RWKV_HEADS = D_MODEL // RWKV_N
RWKV_W = RWKV_HEADS * RWKV_N
RWKV_DECAY_LORA = 64
RWKV_AAA_LORA = 64
RWKV_GATE_LORA = 160
RWKV_LN_EPS = 64e-5

GLA_SPLITS = (GLA_KEY_W, GLA_KEY_W, GLA_VAL_W, GLA_VAL_W, GLA_LORA, GLA_LORA)
RWKV_SPLITS = (RWKV_W, RWKV_W, RWKV_W, RWKV_DECAY_LORA, RWKV_DECAY_LORA, RWKV_AAA_LORA, RWKV_GATE_LORA)
GLA_COLS = sum(GLA_SPLITS)
RWKV_COLS = sum(RWKV_SPLITS)
BRANCH_GATE_COLS = 2 * D_MODEL
IN_COLS = GLA_COLS + RWKV_COLS + BRANCH_GATE_COLS

PEER_HEADS = 8
PEER_N_KEYS = 128
PEER_EXPERTS = PEER_N_KEYS * PEER_N_KEYS
PEER_HALF = 128
PEER_TOPK = 16
PEER_BLOCK = 128

kernel_name = 'hybrid_gla_rwkv7_peer_prefix_dit'


def _split(z, sizes):
    return jnp.split(z, np.cumsum(sizes)[:-1].tolist(), axis=-1)


def _rmsnorm(x, w, eps=NORM_EPS):
    xf = x.astype(jnp.float32)
    y = xf * lax.rsqrt(jnp.mean(xf * xf, axis=-1, keepdims=True) + eps)
    return (y * w.astype(jnp.float32)).astype(x.dtype)


def _modulate(h, shift, scale):
    return h * (1 + scale) + shift


def _grid_shift(z, rows):
    b, l, ch = z.shape
    zg = z.reshape(b, rows, GRID_W, ch // 4, 4)
    left = jnp.pad(zg[:, :, :-1, :, 0], ((0, 0), (0, 0), (1, 0), (0, 0)))
    right = jnp.pad(zg[:, :, 1:, :, 1], ((0, 0), (0, 0), (0, 1), (0, 0)))
    up = jnp.pad(zg[:, :-1, :, :, 2], ((0, 0), (1, 0), (0, 0), (0, 0)))
    down = jnp.pad(zg[:, 1:, :, :, 3], ((0, 0), (0, 1), (0, 0), (0, 0)))
    return jnp.stack([left, right, up, down], axis=-1).reshape(b, l, ch)


def _seq_shift(z):
    b, l, ch = z.shape
    zs = z.reshape(b, l, ch // 2, 2)
    prev = jnp.pad(zs[:, :-1, :, 0], ((0, 0), (1, 0), (0, 0)))
    nxt = jnp.pad(zs[:, 1:, :, 1], ((0, 0), (0, 1), (0, 0)))
    return jnp.stack([prev, nxt], axis=-1).reshape(b, l, ch)


def _gla_scan(q, k, v, log_a, s0):
    b, l, h, _ = q.shape
    n = l // GLA_CHUNK

    def chunks(t):
        return t.reshape(b, n, GLA_CHUNK, h, t.shape[-1]).transpose(1, 0, 3, 2, 4)

    mask = jnp.tril(jnp.ones((GLA_CHUNK, GLA_CHUNK), dtype=bool))

    def step(s, xs):
        qc, kc, vc, gc = xs
        bcum = jnp.cumsum(gc, axis=-2)
        blast = bcum[..., -1:, :]
        q_dec = qc * jnp.exp(bcum)
        k_dec = kc * jnp.exp(-bcum)
        att = jnp.where(mask, jnp.einsum('bhid,bhjd->bhij', q_dec, k_dec), 0.0)
        o = jnp.einsum('bhid,bhde->bhie', q_dec, s) + jnp.einsum('bhij,bhje->bhie', att, vc)
        s_new = (jnp.exp(blast[..., 0, :])[..., None] * s
                 + jnp.einsum('bhjd,bhje->bhde', kc * jnp.exp(blast - bcum), vc))
        return s_new, o

    s_fin, o = lax.scan(step, s0, (chunks(q), chunks(k), chunks(v), chunks(log_a)))
    o = o.transpose(1, 0, 3, 2, 4).reshape(b, l, h, v.shape[-1])
    return o, s_fin


def _rwkv7_scan(r, decay, k, v, kk, a, s0, reverse):
    xs = tuple(jnp.moveaxis(t, 1, 0) for t in (r, decay, k, v, kk, a))

    def step(s, xt):
        rt, dt, kt, vt, kkt, at = xt
        sa = jnp.einsum('bhvk,bhk->bhv', s, kkt)
        s = (s * dt[:, :, None, :] - sa[..., None] * (kkt * at)[:, :, None, :]
             + vt[..., None] * kt[:, :, None, :])
        return s, jnp.einsum('bhvk,bhk->bhv', s, rt)

    s_fin, o = lax.scan(step, s0, xs, reverse=reverse)
    return jnp.moveaxis(o, 0, 1), s_fin


def _token_mixer(hm, rows, init_states, need_out, lp):
    b, l, _ = hm.shape
    f32 = jnp.float32

    def heads(t, nh):
        return t.reshape(b, l, nh, -1).astype(f32)

    def flip(t):
        return jnp.flip(t, axis=1)

    z = hm @ lp['w_in']
    z_gla, z_rwkv, z_gate = _split(z, (GLA_COLS, RWKV_COLS, BRANCH_GATE_COLS))
    sg_f0, sg_b0, sr_f0, sr_b0 = init_states

    q, k, v, g_out, alo_f, alo_b = _split(z_gla, GLA_SPLITS)

    def gla_log_decay(alo, d):
        return jax.nn.log_sigmoid(alo @ lp['gla_w_a2'][d] + lp['gla_b_a'][d]) / GLA_GATE_NORM

    qh = heads(q, GLA_HEADS) * GLA_DK ** -0.5
    kh = heads(k, GLA_HEADS)
    vh = heads(v, GLA_HEADS)
    lf = heads(gla_log_decay(alo_f, 0), GLA_HEADS)
    lb = heads(gla_log_decay(alo_b, 1), GLA_HEADS)
    og_f, sg_f = _gla_scan(qh, kh, vh, lf, sg_f0)
    og_b, sg_b = _gla_scan(flip(qh), flip(kh), flip(vh), flip(lb), sg_b0)

    zs = _seq_shift(z_rwkv) if rows is None else _grid_shift(z_rwkv, rows)
    zm = z_rwkv + (zs - z_rwkv) * lp['rwkv_mu']
    r, kr, vr, wlo_f, wlo_b, alo, glo = _split(zm, RWKV_SPLITS)

    def rwkv_decay(wlo, d):
        w = -jax.nn.softplus(-(lp['rwkv_w0'][d] + jnp.tanh(wlo) @ lp['rwkv_w2'][d])) - 0.5
        return jnp.exp(-jnp.exp(heads(w, RWKV_HEADS)))

    a = jax.nn.sigmoid(lp['rwkv_a0'] + alo @ lp['rwkv_a2'])
    kk = heads(kr * lp['rwkv_k_k'], RWKV_HEADS)
    kk = kk / jnp.maximum(jnp.linalg.norm(kk, axis=-1, keepdims=True), 1e-12)
    kr = kr * (1 + (a - 1) * lp['rwkv_k_a'])
    rh = heads(r, RWKV_HEADS)
    krh = heads(kr, RWKV_HEADS)
    vrh = heads(vr, RWKV_HEADS)
    ah = heads(a, RWKV_HEADS)
    or_f, sr_f = _rwkv7_scan(rh, rwkv_decay(wlo_f, 0), krh, vrh, kk, ah, sr_f0, False)
    or_b, sr_b = _rwkv7_scan(rh, rwkv_decay(wlo_b, 1), krh, vrh, kk, ah, sr_b0, True)
    states = (sg_f, sg_b, sr_f, sr_b)
    if not need_out:
        return None, states

    og = og_f + flip(og_b)
    og = og * lax.rsqrt(jnp.mean(og * og, axis=-1, keepdims=True) + NORM_EPS)
    og = og * lp['gla_norm_w'].reshape(GLA_HEADS, GLA_DV).astype(f32)
    y_gla = og.reshape(b, l, GLA_VAL_W).astype(hm.dtype) * jax.nn.silu(g_out)

    o_r = or_f + or_b
    mu = jnp.mean(o_r, axis=-1, keepdims=True)
    var = jnp.mean(jnp.square(o_r - mu), axis=-1, keepdims=True)
    o_r = ((o_r - mu) * lax.rsqrt(var + RWKV_LN_EPS) * lp['rwkv_ln_w'].reshape(RWKV_HEADS, RWKV_N)
           + lp['rwkv_ln_b'].reshape(RWKV_HEADS, RWKV_N))
    o_r = o_r + jnp.sum(rh * krh * lp['rwkv_r_k'], axis=-1, keepdims=True) * vrh
    y_rwkv = o_r.reshape(b, l, RWKV_W).astype(hm.dtype) * (jax.nn.sigmoid(glo) @ lp['rwkv_g2'])

    gate_gla, gate_rwkv = jnp.split(z_gate, 2, axis=-1)
    y = jax.nn.sigmoid(gate_gla) * y_gla + jax.nn.sigmoid(gate_rwkv) * y_rwkv
    return y @ lp['w_out'], states


def _peer(h, w_q, sub_keys, exp_u, exp_v):
    b, l, d = h.shape
    q = (h @ w_q).reshape(b, l, PEER_HEADS, 2, PEER_HALF)
    s = jnp.einsum('blhpd,hpnd->blhpn', q, sub_keys).astype(jnp.float32)
    s1, i1 = lax.top_k(s[..., 0, :], PEER_TOPK)
    s2, i2 = lax.top_k(s[..., 1, :], PEER_TOPK)
    cand_s = (s1[..., :, None] + s2[..., None, :]).reshape(b, l, PEER_HEADS, PEER_TOPK * PEER_TOPK)
    cand_i = (i1[..., :, None] * PEER_N_KEYS + i2[..., None, :]).reshape(b, l, PEER_HEADS, PEER_TOPK * PEER_TOPK)
    top_s, pos = lax.top_k(cand_s, PEER_TOPK)
    idx = jnp.take_along_axis(cand_i, pos, axis=-1)
    gates = jax.nn.softmax(top_s, axis=-1).astype(h.dtype)
    n_blocks = (b * l) // PEER_BLOCK
    slots = PEER_HEADS * PEER_TOPK
    xs = (h.reshape(n_blocks, PEER_BLOCK, d),
          idx.reshape(n_blocks, PEER_BLOCK, slots),
          gates.reshape(n_blocks, PEER_BLOCK, slots))

    def block(args):
        xt, it, gt = args
        u = jnp.take(exp_u, it, axis=0)
        act = jax.nn.gelu(jnp.einsum('tsd,td->ts', u, xt), approximate=False)
        vv = jnp.take(exp_v, it, axis=0)
        return jnp.einsum('ts,tsd->td', gt * act, vv)

    return lax.map(block, xs).reshape(b, l, d)


def setup_inputs(seed: int = 0) -> dict:
    key = jax.random.key(seed)
    ks = iter(jax.random.split(key, 29))
    D = D_MODEL
    L = DEPTH

    def nrm(shape, scale):
        return scale * jax.random.normal(next(ks), shape, jnp.float32)

    def uni(shape, lo, hi):
        return jax.random.uniform(next(ks), shape, jnp.float32, lo, hi)

    return {
        'x': nrm((BATCH, SEQ, D), 1.0),
        'c': nrm((BATCH, D), 1.0),
        'ctx': nrm((BATCH, CTX_LEN, D), 1.0),
        'c_ctx': nrm((D,), 1.0),
        'norm1_w': 1.0 + nrm((L, D), 0.02),
        'w_mod': nrm((L, D, ADALN_CHUNKS * D), 0.5 * D ** -0.5),
        'b_mod': nrm((L, ADALN_CHUNKS * D), 0.02),
        'w_in': nrm((L, D, IN_COLS), D ** -0.5),
        'gla_w_a2': nrm((L, 2, GLA_LORA, GLA_KEY_W), GLA_LORA ** -0.5),
        'gla_b_a': nrm((L, 2, GLA_KEY_W), 0.1),
        'gla_norm_w': 1.0 + nrm((L, GLA_VAL_W), 0.02),
        'rwkv_mu': uni((L, RWKV_COLS), 0.2, 0.8),
        'rwkv_w0': uni((L, 2, RWKV_W), -6.0, -1.0),
        'rwkv_w2': nrm((L, 2, RWKV_DECAY_LORA, RWKV_W), 0.5 * RWKV_DECAY_LORA ** -0.5),
        'rwkv_a0': nrm((L, RWKV_W), 0.1),
        'rwkv_a2': nrm((L, RWKV_AAA_LORA, RWKV_W), 0.5 * RWKV_AAA_LORA ** -0.5),
        'rwkv_g2': nrm((L, RWKV_GATE_LORA, RWKV_W), RWKV_GATE_LORA ** -0.5),
        'rwkv_k_k': 0.85 + nrm((L, RWKV_W), 0.02),
        'rwkv_k_a': 1.0 + nrm((L, RWKV_W), 0.02),
        'rwkv_r_k': nrm((L, RWKV_HEADS, RWKV_N), 0.1),
        'rwkv_ln_w': 1.0 + nrm((L, RWKV_W), 0.02),
        'rwkv_ln_b': nrm((L, RWKV_W), 0.02),
        'w_out': nrm((L, D, D), D ** -0.5),
        'norm2_w': 1.0 + nrm((L, D), 0.02),
        'peer_w_q': nrm((L, D, PEER_HEADS * 2 * PEER_HALF), D ** -0.5),
        'peer_sub_keys': nrm((L, PEER_HEADS, 2, PEER_N_KEYS, PEER_HALF), PEER_HALF ** -0.5),
        'peer_u': nrm((L, PEER_EXPERTS, D), D ** -0.5),
        'peer_v': nrm((L, PEER_EXPERTS, D), 1.0),
        'final_norm_w': 1.0 + nrm((D,), 0.02),
    }


def reference(x, c, ctx, c_ctx, norm1_w, w_mod, b_mod, w_in, gla_w_a2, gla_b_a, gla_norm_w,
              rwkv_mu, rwkv_w0, rwkv_w2, rwkv_a0, rwkv_a2, rwkv_g2, rwkv_k_k, rwkv_k_a, rwkv_r_k,
              rwkv_ln_w, rwkv_ln_b, w_out, norm2_w, peer_w_q, peer_sub_keys, peer_u, peer_v,
              final_norm_w):
    b, seq, _ = x.shape
    rows = seq // GRID_W
    f32 = jnp.float32
    zero_states = (jnp.zeros((b, GLA_HEADS, GLA_DK, GLA_DV), f32),
                   jnp.zeros((b, GLA_HEADS, GLA_DK, GLA_DV), f32),
                   jnp.zeros((b, RWKV_HEADS, RWKV_N, RWKV_N), f32),
                   jnp.zeros((b, RWKV_HEADS, RWKV_N, RWKV_N), f32))
    h_lat, h_ctx = x, ctx
    for layer in range(DEPTH):
        last = layer == DEPTH - 1
        lp = {
            'w_in': w_in[layer], 'gla_w_a2': gla_w_a2[layer], 'gla_b_a': gla_b_a[layer],
            'gla_norm_w': gla_norm_w[layer], 'rwkv_mu': rwkv_mu[layer], 'rwkv_w0': rwkv_w0[layer],
            'rwkv_w2': rwkv_w2[layer], 'rwkv_a0': rwkv_a0[layer], 'rwkv_a2': rwkv_a2[layer],
            'rwkv_g2': rwkv_g2[layer], 'rwkv_k_k': rwkv_k_k[layer], 'rwkv_k_a': rwkv_k_a[layer],
            'rwkv_r_k': rwkv_r_k[layer], 'rwkv_ln_w': rwkv_ln_w[layer], 'rwkv_ln_b': rwkv_ln_b[layer],
            'w_out': w_out[layer],
        }
        m_lat = jnp.split((jax.nn.silu(c) @ w_mod[layer] + b_mod[layer])[:, None, :], ADALN_CHUNKS, axis=-1)
        m_ctx = jnp.split(jax.nn.silu(c_ctx) @ w_mod[layer] + b_mod[layer], ADALN_CHUNKS, axis=-1)

        a_ctx = _modulate(_rmsnorm(h_ctx, norm1_w[layer]), m_ctx[0], m_ctx[1])
        y_ctx, ctx_states = _token_mixer(a_ctx, None, zero_states, not last, lp)

        a_lat = _modulate(_rmsnorm(h_lat, norm1_w[layer]), m_lat[0], m_lat[1])
        y_lat, _ = _token_mixer(a_lat, rows, ctx_states, True, lp)
        h_lat = h_lat + m_lat[2] * y_lat
        f_lat = _peer(_modulate(_rmsnorm(h_lat, norm2_w[layer]), m_lat[3], m_lat[4]),
                      peer_w_q[layer], peer_sub_keys[layer], peer_u[layer], peer_v[layer])
        h_lat = h_lat + m_lat[5] * f_lat

        if not last:
            h_ctx = h_ctx + m_ctx[2] * y_ctx
            f_ctx = _peer(_modulate(_rmsnorm(h_ctx, norm2_w[layer]), m_ctx[3], m_ctx[4]),
                          peer_w_q[layer], peer_sub_keys[layer], peer_u[layer], peer_v[layer])
            h_ctx = h_ctx + m_ctx[5] * f_ctx
    return _rmsnorm(h_lat, final_norm_w)
```

```python
import numpy as np
from contextlib import ExitStack
import concourse.bass as bass
import concourse.mybir as mybir
from concourse.bass_utils import run_bass_kernel_spmd

F32 = mybir.dt.float32
BF16 = mybir.dt.bfloat16
F32R = mybir.dt.float32r
FAST32 = True
I32 = mybir.dt.int32
U32 = mybir.dt.uint32
ALU = mybir.AluOpType
AF = mybir.ActivationFunctionType
AX = mybir.AxisListType

D = 1024
SEQ = 2048
CTX = 256
NTOK = SEQ + CTX
NT = NTOK // 128
INC = 8576
GLA0, RW0, GATE0 = 0, 3104, 6528
RWC = 3424
C0 = 0.6065306597126334
NE = 16384


class Sched:
    LIM = 3500
    DLIM = 3072

    def __init__(self, nc, stack, ndma=16):
        self.nc = nc
        self.stack = stack
        self.eng = dict(pe=nc.tensor, dve=nc.vector, act=nc.scalar, pool=nc.gpsimd, sp=nc.sync)
        self.semobj = {}
        self.nsem = 0
        self.cur = {}
        self.cnt = {}
        self.ep = {}
        for e in self.eng:
            self.ep[e] = 0
            self.cur[e] = (e, 0)
            self.semobj[self.cur[e]] = self._newsem()
            self.cnt[e] = 0
        self.nhw = ndma
        self.nsw = 10
        ndma = self.nhw + self.nsw
        self.ndma = ndma
        self.dcur = []
        self.dval = [0] * ndma
        self.dep = [0] * ndma
        for i in range(ndma):
            k = ('d', i, 0)
            self.semobj[k] = self._newsem()
            self.dcur.append(k)
        self.dpend = {}
        self.dnext = 0
        self.dnext_sw = 0
        self.seen = {e: {} for e in self.eng}
        self.state = {}
        self.ninst = 0

    def _newsem(self):
        self.nsem += 1
        return self.stack.enter_context(self.nc.semaphore("sm%d" % self.nsem))

    def _deps(self, reads, writes):
        deps = {}

        def add(ev):
            if ev is not None and deps.get(ev[0], 0) < ev[1]:
                deps[ev[0]] = ev[1]
        for key in reads:
            st = self.state.get(key)
            if st is not None:
                add(st[0])
        for key in writes:
            st = self.state.get(key)
            if st is not None:
                add(st[0])
                for k, v in st[1].items():
                    add((k, v))
        return deps

    def _wait(self, eng, deps):
        seen = self.seen[eng]
        for k, v in deps.items():
            if k[0] == 'pe' and eng == 'pe':
                continue
            if seen.get(k, 0) >= v:
                continue
            self.eng[eng].wait_ge(self.semobj[k], v)
            self.ninst += 1
            seen[k] = v

    def _record(self, ev, reads, writes):
        for key in reads:
            st = self.state.setdefault(key, [None, {}])
            if st[1].get(ev[0], 0) < ev[1]:
                st[1][ev[0]] = ev[1]
        for key in writes:
            self.state[key] = [ev, {}]

    def op(self, eng, fn, reads=(), writes=()):
        psr = [k for k in reads if isinstance(k, str) and k.startswith('ps') and k[2:].isdigit()]
        if psr:
            reads = [k for k in reads if k not in psr]
            writes = list(writes) + [k for k in psr if k not in writes]
        self._wait(eng, self._deps(reads, writes))
        ins = fn(self.eng[eng])
        self.cnt[eng] += 1
        k = self.cur[eng]
        ins.then_inc(self.semobj[k], 1)
        self.ninst += 1
        self._record((k, self.cnt[eng]), reads, writes)
        if self.cnt[eng] >= self.LIM:
            self.ep[eng] += 1
            self.cur[eng] = (eng, self.ep[eng])
            self.semobj[self.cur[eng]] = self._newsem()
            self.cnt[eng] = 0
            self.prev_last = getattr(self, 'prev_last', {})
            self.prev_last[eng] = (k, self.LIM)
        return ins

    def dma(self, q, out, in_, reads=(), writes=(), indirect=None, **kw):
        deps = self._deps(reads, writes)
        if q == 'pool':
            i = self.nhw + self.dnext_sw
            self.dnext_sw = (self.dnext_sw + 1) % self.nsw
        else:
            i = self.dnext
            self.dnext = (self.dnext + 1) % self.nhw
        if self.dval[i] >= self.DLIM:
            self.dpend[self.dcur[i]] = self.dval[i]
            self.dep[i] += 1
            self.dcur[i] = ('d', i, self.dep[i])
            self.semobj[self.dcur[i]] = self._newsem()
            self.dval[i] = 0
        k = self.dcur[i]
        if self.dval[i] > 0 and deps.get(k, 0) < self.dval[i]:
            deps[k] = self.dval[i]
        self._wait(q, deps)
        e = self.eng[q]
        if indirect is None:
            ins = e.dma_start(out=out, in_=in_, **kw)
        else:
            ins = e.indirect_dma_start(out=out, out_offset=None, in_=in_, in_offset=indirect)
        self.dval[i] += 16
        ins.then_inc(self.semobj[k], 16)
        self.ninst += 1
        self._record((k, self.dval[i]), reads, writes)
        return ins

    def barrier(self):
        for e in self.eng:
            deps = {}
            for f in self.eng:
                if f == e and e == 'pe':
                    continue
                if self.cnt[f] > 0:
                    deps[self.cur[f]] = self.cnt[f]
                elif self.ep[f] > 0:
                    deps[(f, self.ep[f] - 1)] = self.LIM
            for i in range(self.ndma):
                if self.dval[i] > 0:
                    deps[self.dcur[i]] = self.dval[i]
            for k, v in self.dpend.items():
                deps[k] = v
            self._wait(e, deps)
        self.dpend = {}


def build_nc(dbg=(), stop=None, rstop=0):
    nc = bass.Bass("TRN2", target_bir_lowering=False)

    def din(name, shape, dt=F32):
        return nc.dram_tensor(name, list(shape), dt, kind="ExternalInput").ap()

    def dscr(name, shape, dt=F32):
        return nc.dram_tensor(name, list(shape), dt, kind="Internal").ap()

    x_d = din("x", [SEQ, D]); ctx_d = din("ctx", [CTX, D]); c2_d = din("c2", [2, D])
    n1w_d = din("norm1_w", [D]); wmod_d = din("w_mod", [D, 6 * D]); bmod_d = din("b_mod", [6 * D])
    win_d = din("w_in", [D, INC]); wa_d = din("gla_wa", [33, 1024]); gnw_d = din("gla_norm_w", [D])
    mu_d = din("rwkv_mu", [RWC]); w2a_d = din("rwkv_w2a", [2, 65, D]); a2a_d = din("rwkv_a2a", [65, D])
    g2_d = din("rwkv_g2", [160, D]); kk_d = din("rwkv_k_k", [D]); ka_d = din("rwkv_k_a", [D])
    rk_d = din("rwkv_r_k", [D]); lnw_d = din("rwkv_ln_w", [D]); lnb_d = din("rwkv_ln_b", [D])
    wout_d = din("w_out", [D, D]); n2w_d = din("norm2_w", [D]); wq_d = din("peer_w_q", [D, 2048])
    sk_d = din("peer_sub_keys", [16, 128, 128]); pu_d = din("peer_u", [NE, D]); pv_d = din("peer_v", [NE, D])
    fnw_d = din("final_norm_w", [D])
    cst_d = din("cst", [128, 1184])
    out_d = nc.dram_tensor("out", [SEQ, D], F32, kind="ExternalOutput").ap()
    dbg_d = {}
    for name, shape in dbg:
        dbg_d[name] = nc.dram_tensor("dbg_" + name, list(shape), F32, kind="ExternalOutput").ap()

    m_scr = dscr("m_scr", [2, 6 * D])
    z_scr = dscr("z_scr", [NTOK, INC])
    ogf_scr = dscr("ogf_scr", [SEQ, D])
    orf_scr = dscr("orf_scr", [SEQ, D])
    ygla_scr = dscr("ygla_scr", [SEQ, D])
    ymix_scr = dscr("ymix_scr", [SEQ, D])
    kkb_scr = dscr("kkb_scr", [NTOK, 3 * D])
    zm_scr = dscr("zm_scr", [NTOK, RWC])
    uv16_scr = dscr("uv16_scr", [NE, 2 * D], BF16)

    with ExitStack() as top:
        S = Sched(nc, top)
        op, dma = S.op, S.dma

        def sbt(st, name, shape, dt=F32):
            return st.enter_context(nc.sbuf_tensor(name, list(shape), dt))

        psA = top.enter_context(nc.psum_tensor("psA", [128, 2048], F32))
        psB = top.enter_context(nc.psum_tensor("psB", [128, 2048], F32))

        def bank(i):
            t = psA if i < 4 else psB
            j = i % 4
            return t[:, j * 512:(j + 1) * 512], "ps%d" % i

        def pkeys(lo, n):
            return ["ps%d" % i for i in range(lo, lo + n)]

        cst = sbt(top, "cst_sb", [128, 1184])
        dma('sp', cst[:], cst_d, writes=['cst'])
        ident = cst[:, 0:128]
        M_le = cst[:, 128:256]; M_lt = cst[:, 256:384]; M_ge = cst[:, 384:512]; M_gt = cst[:, 512:640]
        gcum = [cst[:, 640:768], cst[:, 768:896]]
        rcum = [cst[:, 896:1024], cst[:, 1024:1152]]
        shm = cst[:, 1152:1168]
        iota16 = cst[:, 1168:1184]

        def mm(out, lhsT, rhs, R, W, start=True, stop=True, r32=False):
            if r32 and FAST32:
                if lhsT.dtype != F32R:
                    lhsT = lhsT.bitcast(F32R)
                if rhs.dtype != F32R:
                    rhs = rhs.bitcast(F32R)
            else:
                if lhsT.dtype == F32R:
                    lhsT = lhsT.bitcast(F32)
                if rhs.dtype == F32R:
                    rhs = rhs.bitcast(F32)
            op('pe', lambda e: e.matmul(out=out, lhsT=lhsT, rhs=rhs, start=start, stop=stop),
               reads=list(R) + ['cst'], writes=W)

        def tr(out, in_, R, W, n=128):
            op('pe', lambda e: e.transpose(out=out, in_=in_, identity=ident[0:n, 0:n]), reads=list(R) + ['cst'], writes=W)

        def act(out, in_, func, R, W, **kw):
            op('act', lambda e: e.activation(out=out, in_=in_, func=func, **kw), reads=R, writes=W)

        def tt(eng, out, in0, in1, o, R, W):
            op(eng, lambda e: e.tensor_tensor(out=out, in0=in0, in1=in1, op=o), reads=R, writes=W)

        def ts(eng, out, in0, s1, s2, o0, o1, R, W):
            if s2 is None:
                op(eng, lambda e: e.tensor_scalar(out=out, in0=in0, scalar1=s1, scalar2=None, op0=o0), reads=R, writes=W)
            else:
                op(eng, lambda e: e.tensor_scalar(out=out, in0=in0, scalar1=s1, scalar2=s2, op0=o0, op1=o1), reads=R, writes=W)

        def stt(eng, out, in0, sc, in1, o0, o1, R, W):
            op(eng, lambda e: e.scalar_tensor_tensor(out=out, in0=in0, scalar=sc, in1=in1, op0=o0, op1=o1), reads=R, writes=W)

        def cp(eng, out, in_, R, W):
            op(eng, lambda e: e.tensor_copy(out=out, in_=in_), reads=R, writes=W)

        def red(out, in_, R, W):
            op('dve', lambda e: e.tensor_reduce(out=out, in_=in_, axis=AX.X, op=ALU.add), reads=R, writes=W)

        def rstd_from(ssum, n, eps, key):
            ts('dve', ssum, ssum, 1.0 / n, eps, ALU.mult, ALU.add, [key], [key])
            act(ssum, ssum, AF.Sqrt, [key], [key])
            op('dve', lambda e: e.reciprocal(out=ssum, in_=ssum), reads=[key], writes=[key])

        def dump(name, ap, key):
            if name in dbg_d:
                dma('sp', dbg_d[name], ap, reads=[key])

        def tokrows(i):
            return slice(i * 128, (i + 1) * 128)

        with ExitStack() as st:
            c1 = sbt(st, "c1", [128, 8, 2]); sc = sbt(st, "sc", [128, 8, 2])
            bm = sbt(st, "bm", [2, 6 * D]); mrow = sbt(st, "mrow", [2, 6 * D])
            wb = [sbt(st, "wmb%d" % i, [128, 8, 512]) for i in range(2)]
            for j in range(2):
                dma('sp', c1[:, :, j], c2_d[j, :].rearrange("(k p) -> p k", p=128), writes=['c1'], allow_slow_non_contiguous=True)
            dma('sp', bm[:], bmod_d.partition_broadcast(2), writes=['bm'])
            act(sc[:], c1[:], AF.Silu, ['c1'], ['sc'])
            for nb in range(12):
                w = wb[nb % 2]; wk = "wmb%d" % (nb % 2)
                dma('sp' if nb % 2 == 0 else 'act', w[:], wmod_d[:, nb * 512:(nb + 1) * 512].rearrange("(k p) n -> p k n", p=128), writes=[wk])
                pb, pk = bank(nb % 2)
                for k in range(8):
                    mm(pb[0:2, :], sc[:, k, :], w[:, k, :], ['sc', wk], [pk], start=(k == 0), stop=(k == 7))
                tt('dve', mrow[:, nb * 512:(nb + 1) * 512], pb[0:2, :], bm[:, nb * 512:(nb + 1) * 512], ALU.add, [pk, 'bm'], ['mrow'])
            dma('sp', m_scr, mrow[:], reads=['mrow'], writes=['m_scr'])
        S.barrier()

        with ExitStack() as st:
            AT = sbt(st, "AT", [128, 8, NTOK], BF16)
            with ExitStack() as st1:
                fm = sbt(st1, "fm", [128, 5, 8])
                gs = sbt(st1, "gs", [128, 2, 8])
                dma('sp', fm[:, 0, :], n1w_d.rearrange("(k p) -> p k", p=128), writes=['fm'], allow_slow_non_contiguous=True)
                for j, (r, c) in enumerate([(0, 0), (0, 1), (1, 0), (1, 1)]):
                    dma('sp', fm[:, 1 + j, :], m_scr[r, c * D:(c + 1) * D].rearrange("(k p) -> p k", p=128), reads=['m_scr'], writes=['fm'], allow_slow_non_contiguous=True)
                stt('dve', gs[:, 0, :], fm[:, 2, :], 1.0, fm[:, 0, :], ALU.add, ALU.mult, ['fm'], ['gs'])
                stt('dve', gs[:, 1, :], fm[:, 4, :], 1.0, fm[:, 0, :], ALU.add, ALU.mult, ['fm'], ['gs'])
                xt = [sbt(st1, "xt%d" % i, [128, D]) for i in range(2)]
                junk = sbt(st1, "junk1", [128, D]); tmp = sbt(st1, "tmp1", [128, 8, 128])
                ss = sbt(st1, "ss1", [128, 2])
                for i in range(NT):
                    b = i % 2; xk = "xt%d" % b
                    src = ctx_d[tokrows(i), :] if i < 2 else x_d[tokrows(i - 2), :]
                    dma('sp', xt[b][:], src, writes=[xk])
                    sk = "ss1_%d" % b
                    act(junk[:], xt[b][:], AF.Square, [xk], ['junk1', sk], accum_out=ss[:, b:b + 1])
                    rstd_from(ss[:, b:b + 1], D, 1e-6, sk)
                    ts('dve', xt[b][:], xt[b][:], ss[:, b:b + 1], None, ALU.mult, None, [xk, sk], [xk])
                    for k in range(8):
                        pb, pk = bank((i % 2) * 2 + k // 4)
                        tr(pb[:, (k % 4) * 128:(k % 4 + 1) * 128], xt[b][:, k * 128:(k + 1) * 128], [xk], [pk])
                    g = 1 if i < 2 else 0
                    sh = 3 if i < 2 else 1
                    for hlf in range(2):
                        pb, pk = bank((i % 2) * 2 + hlf)
                        pv = pb.rearrange("p (k t) -> p k t", k=4)
                        tt('dve', tmp[:, hlf * 4:(hlf + 1) * 4, :], pv, gs[:, g, hlf * 4:(hlf + 1) * 4].unsqueeze(2).to_broadcast([128, 4, 128]),
                           ALU.mult, [pk, 'gs'], ['tmp1'])
                        tt('dve', AT[:, hlf * 4:(hlf + 1) * 4, tokrows(i)], tmp[:, hlf * 4:(hlf + 1) * 4, :],
                           fm[:, sh, hlf * 4:(hlf + 1) * 4].unsqueeze(2).to_broadcast([128, 4, 128]), ALU.add, ['tmp1', 'fm'], ['AT'])
            S.barrier()
            with ExitStack() as st2:
                wf = [sbt(st2, "wf%d" % i, [128, 8, 512]) for i in range(2)]
                wbf = [sbt(st2, "wbf%d" % i, [128, 8, 512], BF16) for i in range(2)]
                zt = [sbt(st2, "zt%d" % i, [128, 512]) for i in range(4)]
                cnt = 0
                for nb in range(17):
                    n0 = nb * 512; nw = min(512, INC - n0)
                    b = nb % 2
                    dma('sp' if b == 0 else 'act', wf[b][:, :, 0:nw], win_d[:, n0:n0 + nw].rearrange("(k p) n -> p k n", p=128), writes=["wf%d" % b])
                    cp('pool', wbf[b][:, 0:4, 0:nw], wf[b][:, 0:4, 0:nw], ["wf%d" % b], ["wbf%d" % b])
                    cp('dve', wbf[b][:, 4:8, 0:nw], wf[b][:, 4:8, 0:nw], ["wf%d" % b], ["wbfb%d" % b])
                    for i in range(NT):
                        pb, pk = bank(cnt % 8)
                        for k in range(8):
                            mm(pb[:, 0:nw], AT[:, k, tokrows(i)], wbf[b][:, k, 0:nw], ['AT', "wbf%d" % b, "wbfb%d" % b], [pk], start=(k == 0), stop=(k == 7))
                        zb = cnt % 4
                        if cnt % 2 == 0:
                            act(zt[zb][:, 0:nw], pb[:, 0:nw], AF.Identity, [pk], ["zt%d" % zb])
                        else:
                            cp('dve', zt[zb][:, 0:nw], pb[:, 0:nw], [pk], ["zt%d" % zb])
                        dma('sp', z_scr[tokrows(i), n0:n0 + nw], zt[zb][:, 0:nw], reads=["zt%d" % zb], writes=[])
                        cnt += 1
        S.barrier()
        if 'z' in dbg_d:
            for i in range(NT):
                dma('sp', dbg_d['z'][tokrows(i), :], z_scr[tokrows(i), :])
            S.barrier()
        if stop == 'p2':
            return nc

        with ExitStack() as st:
            WA = sbt(st, "WA", [33, 1024]); gnw = sbt(st, "gnw", [128, D])
            dma('sp', WA[:], wa_d, writes=['WA'])
            dma('sp', gnw[:], gnw_d.partition_broadcast(128), writes=['gnw'])
            zqk = sbt(st, "zqk", [128, 1024]); zv = sbt(st, "zv", [128, 1024]); zalo = sbt(st, "zalo", [128, 32])
            aloT = sbt(st, "aloT", [33, 128])
            op('dve', lambda e: e.memset(aloT[:], 1.0), writes=['aloT'])
            e1 = sbt(st, "e1", [128, 512]); sp_ = sbt(st, "sp", [128, 512]); Em = sbt(st, "Em", [128, 512])
            EpT = sbt(st, "EpT", [128, 512]); EmT = sbt(st, "EmT", [128, 512]); el = sbt(st, "el", [128, 4])
            kd = sbt(st, "kd", [128, 512], BF16); qdT = sbt(st, "qdT", [128, 512], BF16); kdT = sbt(st, "kdT", [128, 512], BF16)
            vb = sbt(st, "vb", [128, 1024], BF16); attb = sbt(st, "attb", [128, 512], BF16)
            Sf = [sbt(st, "Sf%d" % d, [128, 1024]) for d in range(2)]
            Sb = [sbt(st, "Sb%d" % d, [128, 1024], BF16) for d in range(2)]
            og = sbt(st, "og", [128, 1024]); ogf = sbt(st, "ogf", [128, 1024]); sq = sbt(st, "sqg", [128, 1024])
            zg = sbt(st, "zg", [128, 1024]); zgg = sbt(st, "zgg", [128, 1024]); rs4 = sbt(st, "rs4", [128, 4])
            for d in range(2):
                op('dve', lambda e: e.memset(Sf[d][:], 0.0), writes=["Sf%d" % d])
                op('pool', lambda e: e.memset(Sb[d][:], 0.0), writes=["Sb%d" % d])
            for d in range(2):
                order = list(range(NT)) if d == 0 else [1, 0] + list(range(NT - 1, 1, -1))
                last = 127 if d == 0 else 0
                Matt = M_le if d == 0 else M_ge
                SfK, SbK = "Sf%d" % d, "Sb%d" % d
                for i in order:
                    lat = i >= 2
                    li = i - 2
                    dma('sp', zqk[:], z_scr[tokrows(i), 0:1024], reads=[], writes=['zqk'])
                    dma('act', zv[:], z_scr[tokrows(i), 1024:2048], reads=[], writes=['zv'])
                    dma('sp', zalo[:], z_scr[tokrows(i), 3072:3104], reads=[], writes=['zalo'])
                    if lat and d == 1:
                        dma('act', zg[:], z_scr[tokrows(i), 2048:3072], reads=[], writes=['zg'])
                        dma('sp', zgg[:], z_scr[tokrows(i), GATE0:GATE0 + 1024], reads=[], writes=['zgg'])
                        dma('act', ogf[:], ogf_scr[tokrows(li), :], reads=['ogf%d' % li], writes=['ogf'])
                    p0, k0 = bank(0)
                    tr(p0[0:32, 0:128], zalo[:, 0:32], ['zalo'], [k0])
                    cp('dve', aloT[0:32, :], p0[0:32, 0:128], [k0], ['aloT'])
                    p1, k1 = bank(1)
                    mm(p1, aloT[:], WA[:, d * 512:(d + 1) * 512], ['aloT', 'WA'], [k1])
                    act(e1[:], p1, AF.Exp, [k1], ['e1'], scale=-1.0)
                    act(sp_[:], e1[:], AF.Ln, ['e1'], ['sp'], bias=1.0)
                    p2, k2 = bank(2)
                    mm(p2, gcum[d], sp_[:], ['sp'], [k2])
                    act(Em[:], p2, AF.Exp, [k2], ['Em'], scale=-1.0)
                    tt('dve', kd[:], zqk[:, 512:1024], Em[:], ALU.mult, ['zqk', 'Em'], ['kd'])
                    p3, k3 = bank(3)
                    for h in range(4):
                        mm(p3[:, h * 128:(h + 1) * 128], sp_[:, h * 128:(h + 1) * 128], gcum[d], ['sp'], [k3])
                    act(EpT[:], p3, AF.Exp, [k3], ['EpT'])
                    act(EmT[:], p3, AF.Exp, [k3], ['EmT'], scale=-1.0)
                    cp('dve', el[:], EpT[:].rearrange("p (h t) -> p h t", h=4)[:, :, last], ['EpT'], ['el'])
                    p4, k4 = bank(4); p5, k5 = bank(5)
                    for h in range(4):
                        tr(p4[:, h * 128:(h + 1) * 128], zqk[:, h * 128:(h + 1) * 128], ['zqk'], [k4])
                        tr(p5[:, h * 128:(h + 1) * 128], zqk[:, 512 + h * 128:512 + (h + 1) * 128], ['zqk'], [k5])
                    stt('dve', qdT[:], p4, float(128 ** -0.5), EpT[:], ALU.mult, ALU.mult, [k4, 'EpT'], ['qdT'])
                    tt('dve', kdT[:], p5, EmT[:], ALU.mult, [k5, 'EmT'], ['kdT'])
                    cp('pool', vb[:], zv[:], ['zv'], ['vb'])
                    if lat:
                        p6, k6 = bank(6)
                        for h in range(4):
                            mm(p6[:, h * 128:(h + 1) * 128], kdT[:, h * 128:(h + 1) * 128], qdT[:, h * 128:(h + 1) * 128], ['kdT', 'qdT'], [k6])
                        tt('dve', attb[:].rearrange("p (h t) -> p h t", h=4), p6.rearrange("p (h t) -> p h t", h=4),
                           Matt.unsqueeze(1).to_broadcast([128, 4, 128]), ALU.mult, [k6, 'cst'], ['attb'])
                        for h in range(4):
                            pb, pk = bank(h // 2)
                            o_ap = pb[:, (h % 2) * 256:(h % 2 + 1) * 256]
                            mm(o_ap, attb[:, h * 128:(h + 1) * 128], vb[:, h * 256:(h + 1) * 256], ['attb', 'vb'], [pk], start=True, stop=False)
                            mm(o_ap, qdT[:, h * 128:(h + 1) * 128], Sb[d][:, h * 256:(h + 1) * 256], ['qdT', SbK], [pk], start=False, stop=True)
                        if d == 0:
                            cp('dve', og[:, 0:512], bank(0)[0], ['ps0'], ['og'])
                            act(og[:, 512:1024], bank(1)[0], AF.Identity, ['ps1'], ['og'])
                            dma('sp', ogf_scr[tokrows(li), :], og[:], reads=['og'], writes=['ogf%d' % li])
                        else:
                            tt('dve', og[:, 0:512], bank(0)[0], ogf[:, 0:512], ALU.add, ['ps0', 'ogf'], ['og'])
                            tt('dve', og[:, 512:1024], bank(1)[0], ogf[:, 512:1024], ALU.add, ['ps1', 'ogf'], ['og'])
                            dump('og_%d' % li, og[:], 'og')
                            tt('pool', sq[:], og[:], og[:], ALU.mult, ['og'], ['sqg'])
                            red(rs4[:], sq[:].rearrange("p (h e) -> p h e", h=4), ['sqg'], ['rs4'])
                            rstd_from(rs4[:], 256, 1e-6, 'rs4')
                            tt('dve', og[:].rearrange("p (h e) -> p h e", h=4), og[:].rearrange("p (h e) -> p h e", h=4),
                               rs4[:].unsqueeze(2).to_broadcast([128, 4, 256]), ALU.mult, ['og', 'rs4'], ['og'])
                            tt('dve', og[:], og[:], gnw[:], ALU.mult, ['og', 'gnw'], ['og'])
                            act(zg[:], zg[:], AF.Silu, ['zg'], ['zg'])
                            act(zgg[:], zgg[:], AF.Sigmoid, ['zgg'], ['zgg'])
                            tt('pool', zg[:], zg[:], zgg[:], ALU.mult, ['zg', 'zgg'], ['zg'])
                            tt('dve', og[:], og[:], zg[:], ALU.mult, ['og', 'zg'], ['og'])
                            dma('sp', ygla_scr[tokrows(li), :], og[:], reads=['og'], writes=['ygla%d' % li])
                    for h in range(4):
                        pb, pk = bank(2 + h // 2)
                        mm(pb[:, (h % 2) * 256:(h % 2 + 1) * 256], kd[:, h * 128:(h + 1) * 128], vb[:, h * 256:(h + 1) * 256], ['kd', 'vb'], [pk])
                    tt('dve', Sf[d][:, 0:512], bank(2)[0], Sf[d][:, 0:512], ALU.add, ['ps2', SfK], [SfK])
                    tt('dve', Sf[d][:, 512:1024], bank(3)[0], Sf[d][:, 512:1024], ALU.add, ['ps3', SfK], [SfK])
                    tt('dve', Sf[d][:].rearrange("p (h e) -> p h e", h=4), Sf[d][:].rearrange("p (h e) -> p h e", h=4),
                       el[:].unsqueeze(2).to_broadcast([128, 4, 256]), ALU.mult, [SfK, 'el'], [SfK])
                    act(Sb[d][:], Sf[d][:], AF.Identity, [SfK], [SbK])
                    if i == 0 and d == 1:
                        dump('sg_b', Sf[1][:], SfK)
                    if i == 1 and d == 0:
                        dump('sg_f', Sf[0][:], SfK)
        S.barrier()
        if 'ygla' in dbg_d:
            for i in range(16):
                dma('sp', dbg_d['ygla'][tokrows(i), :], ygla_scr[tokrows(i), :])
            S.barrier()
        if stop == 'p3':
            return nc

        with ExitStack() as st:
            W2A = [sbt(st, "W2A%d" % d, [65, D]) for d in range(2)]
            A2A = sbt(st, "A2A", [65, D]); G2a = sbt(st, "G2a", [128, D]); G2b = sbt(st, "G2b", [32, D])
            for d in range(2):
                dma('sp', W2A[d][:], w2a_d[d], writes=["W2A%d" % d])
            dma('sp', A2A[:], a2a_d, writes=['A2A'])
            dma('sp', G2a[:], g2_d[0:128, :], writes=['G2a']); dma('sp', G2b[:], g2_d[128:160, :], writes=['G2b'])
            bcs = {}
            for nm, src in [('kk', kk_d), ('ka', ka_d), ('rk', rk_d), ('lnw', lnw_d), ('lnb', lnb_d)]:
                bcs[nm] = sbt(st, "bc_" + nm, [128, D])
                dma('act', bcs[nm][:], src.partition_broadcast(128), writes=['bc_' + nm])
            zm = sbt(st, "zm", [128, RWC]); zc = sbt(st, "zc", [128, 856]); muc = sbt(st, "muc", [128, 856])
            shb = [sbt(st, "shb%d" % j, [128, 856]) for j in range(2)]
            for j in range(2):
                op('pool', lambda e: e.memset(shb[j][:], 0.0), writes=["shb%d" % j])
            G = [sbt(st, "g%d" % j, [128, D]) for j in range(13)]
            gk = ["g%d" % j for j in range(13)]
            XT = [sbt(st, "XT%d" % j, [128, 1024], F32R if FAST32 else F32) for j in range(4)]
            for j in range(4):
                op('dve', lambda e: e.memset(XT[j][:].bitcast(F32), 0.0), writes=["XT%d_0" % j, "XT%d_1" % j])
            XYW = [sbt(st, "xyw%d" % j, [128, 1024], F32R if FAST32 else F32) for j in range(6)]
            xk = ["xyw%d" % j for j in range(6)]
            G0 = [sbt(st, "G0_%d" % d, [128, D]) for d in range(2)]
            for d in range(2):
                op('dve', lambda e: e.memset(G0[d][:], 0.0), writes=["G0_%d" % d])
            gam = sbt(st, "gam", [64, 256]); negc = sbt(st, "negc", [128, 16]); tw = sbt(st, "tw", [128, 64]); sg = sbt(st, "sgl", [128, 160])
            lt = sbt(st, "lt", [65, 128]); la = sbt(st, "la", [65, 128])
            op('dve', lambda e: e.memset(negc[:], -C0), writes=['negc'])
            op('dve', lambda e: e.memset(lt[:], 1.0), writes=['lt']); op('dve', lambda e: e.memset(la[:], 1.0), writes=['la'])
            s16 = sbt(st, "s16", [128, 16]); gT0 = sbt(st, "gT0", [128, 128]); gT1 = sbt(st, "gT1", [32, 128])

            cvf = [sbt(st, "cvf%d" % j, [128, 2, D]) for j in range(2)]
            cvb = [sbt(st, "cvb%d" % j, [128, 2, D], BF16) for j in range(2)]
            cv_state = [0]

            def convert_chunks(n):
                for _ in range(n):
                    c = cv_state[0]
                    if c >= 128:
                        return
                    cv_state[0] += 1
                    src, c_off = (pu_d, 0) if c < 64 else (pv_d, D)
                    r0 = (c % 64) * 256
                    dst = uv16_scr[:, c_off:c_off + D]
                    b = c % 2
                    dma('sp', cvf[b][:], src[r0:r0 + 256, :].rearrange("(p r) d -> p r d", r=2), writes=["cvf%d" % b])
                    cp('pool', cvb[b][:], cvf[b][:], ["cvf%d" % b], ["cvb%d" % b])
                    dma('sp', dst[r0:r0 + 256, :].rearrange("(p r) d -> p r d", r=2), cvb[b][:], reads=["cvb%d" % b])

            def preg(b0, nb):
                t = psA if b0 < 4 else psB
                j = b0 % 4
                return t[:, j * 512:(j + nb) * 512], pkeys(b0, nb)

            def v3(ap, h):
                return ap.rearrange("p (h t) -> p h t", h=h)

            for d in range(2):
                order = list(range(NT)) if d == 0 else [1, 0] + list(range(NT - 1, 1, -1))
                Mst = M_lt if d == 0 else M_gt
                MstT = M_gt if d == 0 else M_lt
                Min = M_le if d == 0 else M_ge
                G0k = "G0_%d" % d
                for i in order:
                    lat = i >= 2
                    li = i - 2
                    R0 = i * 128
                    if lat:
                        specs = [(-1, 0), (1, 1), (-64, 2 if li == 0 else 4), (64, 3 if li == 15 else 4)]
                    else:
                        specs = [(-1, 5 if i == 0 else 4), (1, 6 if i == 1 else 4)] * 2
                    if d == 0:
                        for c in range(4):
                            c0 = RW0 + c * 856
                            dma('sp', zc[:], z_scr[R0:R0 + 128, c0:c0 + 856], writes=['zc'])
                            dma('act', muc[:], mu_d[c * 856:(c + 1) * 856].partition_broadcast(128), writes=['muc'])
                            zmc = zm[:, c * 856:(c + 1) * 856]
                            for j in range(4):
                                off, mcol = specs[j]
                                lo = R0 + off
                                clo, chi = max(lo, 0), min(lo + 128, NTOK)
                                sb_ = shb[j % 2]; sbk = "shb%d" % (j % 2)
                                dma('sp' if j % 2 == 0 else 'act', sb_[clo - lo:chi - lo, :], z_scr[clo:chi, c0:c0 + 856], writes=[sbk])
                                stt('dve', zmc.rearrange("p (f j) -> p f j", j=4)[:, :, j], sb_[:].rearrange("p (f j) -> p f j", j=4)[:, :, j],
                                    shm[:, mcol:mcol + 1], zc[:].rearrange("p (f j) -> p f j", j=4)[:, :, j], ALU.mult, ALU.subtract,
                                    [sbk, 'zc', 'cst'], ['zm'])
                            tt('dve', zmc, zmc, muc[:], ALU.mult, ['zm', 'muc'], ['zm'])
                            tt('dve', zmc, zmc, zc[:], ALU.add, ['zm', 'zc'], ['zm'])
                        dma('sp', zm_scr[R0:R0 + 128, :], zm[:], reads=['zm'], writes=['zmscr%d' % i])
                    else:
                        dma('sp', zm[:], zm_scr[R0:R0 + 128, :], reads=['zmscr%d' % i], writes=['zm'])
                    if i == 2 and d == 0:
                        dump('zm_2', zm[:], 'zm')
                    r_ = zm[:, 0:1024]; kr = zm[:, 1024:2048]; vr = zm[:, 2048:3072]
                    convert_chunks(4)

                    if rstop == 1:
                        S.barrier()
                        return nc
                    sig, a_, kk_, km_, b_, Ecum, Eneg, Epv, Rt, Kt, Bt, Pt, tmpb = G
                    act(tw[:], zm[:, 3072 + d * 64:3072 + (d + 1) * 64], AF.Tanh, ['zm'], ['tw'])
                    p0, k0 = bank(0)
                    tr(p0[0:64, 0:128], tw[:], ['tw'], [k0])
                    cp('dve', lt[0:64, :], p0[0:64, 0:128], [k0], ['lt'])
                    tr(p0[0:64, 128:256], zm[:, 3200:3264], ['zm'], [k0])
                    cp('dve', la[0:64, :], p0[0:64, 128:256], [k0], ['la'])
                    for hf in range(2):
                        pb, pk = bank(1 + hf)
                        mm(pb, lt[:], W2A[d][:, hf * 512:(hf + 1) * 512], ['lt', "W2A%d" % d], [pk])
                        act(sig[:, hf * 512:(hf + 1) * 512], pb, AF.Sigmoid, [pk], [gk[0]])
                        pb2, pk2 = bank(3 + hf)
                        mm(pb2, la[:], A2A[:, hf * 512:(hf + 1) * 512], ['la', 'A2A'], [pk2])
                        act(a_[:, hf * 512:(hf + 1) * 512], pb2, AF.Sigmoid, [pk2], [gk[1]])
                    for hf in range(2):
                        pb, pk = bank(5 + hf)
                        hs = slice(hf * 512, (hf + 1) * 512)
                        mm(pb, rcum[d], sig[:, hs], [gk[0]], [pk])
                        act(Ecum[:, hs], pb, AF.Exp, [pk], [gk[5]])
                        act(Eneg[:, hs], pb, AF.Exp, [pk], [gk[6]], scale=-1.0)
                        stt('dve', Epv[:, hs], sig[:, hs], C0, pb, ALU.mult, ALU.add, [gk[0], pk], [gk[7]])
                    act(Epv[:], Epv[:], AF.Exp, [gk[7]], [gk[7]])
                    p7, k7 = bank(7)
                    for h in range(16):
                        mm(p7[0:64, 16 * h:16 * h + 16], sig[:, h * 64:(h + 1) * 64], negc[:], [gk[0], 'negc'], [k7])
                    act(gam[:], p7[0:64, 0:256], AF.Exp, [k7], ['gam'])

                    if rstop == 2:
                        S.barrier()
                        return nc
                    if d == 0:
                        tt('dve', kk_[:], kr, bcs['kk'][:], ALU.mult, ['zm', 'bc_kk'], [gk[2]])
                        tt('dve', tmpb[:], kk_[:], kk_[:], ALU.mult, [gk[2]], [gk[12]])
                        red(s16[:], v3(tmpb[:], 16), [gk[12]], ['s16'])
                        act(s16[:], s16[:], AF.Sqrt, ['s16'], ['s16'])
                        ts('dve', s16[:], s16[:], 1e-12, None, ALU.max, None, ['s16'], ['s16'])
                        op('dve', lambda e: e.reciprocal(out=s16[:], in_=s16[:]), reads=['s16'], writes=['s16'])
                        tt('dve', v3(kk_[:], 16), v3(kk_[:], 16), s16[:].unsqueeze(2).to_broadcast([128, 16, 64]), ALU.mult, [gk[2], 's16'], [gk[2]])
                        stt('dve', km_[:], a_[:], -1.0, bcs['ka'][:], ALU.add, ALU.mult, [gk[1], 'bc_ka'], [gk[3]])
                        stt('dve', km_[:], km_[:], 1.0, kr, ALU.add, ALU.mult, [gk[3], 'zm'], [gk[3]])
                        tt('dve', b_[:], a_[:], kk_[:], ALU.mult, [gk[1], gk[2]], [gk[4]])
                        for q_, (buf_, key_) in enumerate([(kk_, gk[2]), (km_, gk[3]), (b_, gk[4])]):
                            dma('sp', kkb_scr[R0:R0 + 128, q_ * D:(q_ + 1) * D], buf_[:], reads=[key_], writes=['kkb%d_%d' % (i, q_)])
                    else:
                        for q_, (buf_, key_) in enumerate([(kk_, gk[2]), (km_, gk[3]), (b_, gk[4])]):
                            dma('sp' if q_ != 1 else 'act', buf_[:], kkb_scr[R0:R0 + 128, q_ * D:(q_ + 1) * D], reads=['kkb%d_%d' % (i, q_)], writes=[key_])
                    tt('dve', Rt[:], r_, Ecum[:], ALU.mult, ['zm', gk[5]], [gk[8]])
                    tt('dve', Kt[:], km_[:], Eneg[:], ALU.mult, [gk[3], gk[6]], [gk[9]])
                    tt('dve', Bt[:], b_[:], Eneg[:], ALU.mult, [gk[4], gk[6]], [gk[10]])
                    tt('dve', Pt[:], kk_[:], Epv[:], ALU.mult, [gk[2], gk[7]], [gk[11]])

                    if rstop == 3:
                        S.barrier()
                        return nc
                    AK, RK, RB, Xin, Uneg, o_ = G[0], G[1], G[2], G[4], G[5], G[6]
                    AKk, RKk, RBk, Xink, Unk, ok = gk[0], gk[1], gk[2], gk[4], gk[5], gk[6]
                    for hh in range(2):
                        for q, (src, sk_) in enumerate([(Rt, gk[8]), (Kt, gk[9]), (Bt, gk[10]), (Pt, gk[11])]):
                            reg, rks = preg(2 * q, 2)
                            for j in range(8):
                                h = hh * 8 + j
                                tr(reg[0:64, j * 128:(j + 1) * 128], src[:, h * 64:(h + 1) * 64], [sk_], [rks[j // 4]])
                            for bnk in range(2):
                                bs = slice(bnk * 512, (bnk + 1) * 512)
                                if q % 2 == 0:
                                    act(XT[q][0:64, bs], reg[0:64, bs], AF.Identity, [rks[bnk]], ["XT%d_%d" % (q, bnk)])
                                else:
                                    cp('dve', XT[q][0:64, bs], reg[0:64, bs], [rks[bnk]], ["XT%d_%d" % (q, bnk)])

                        if rstop == 4:
                            S.barrier()
                            return nc
                        RT, KT, BT, PT = XT
                        Xa, Xb, Ya, Yb, Wa, Wb = XYW

                        def hs128(ap, j):
                            return ap[:, j * 128:(j + 1) * 128]
                        plan = [(0, BT, PT, 'XT2', 'XT3', Xa, xk[0], Mst), (2, PT, BT, 'XT3', 'XT2', Ya, xk[2], MstT),
                                (4, KT, PT, 'XT1', 'XT3', AK, AKk, Mst)]
                        if lat:
                            plan += [(6, KT, RT, 'XT1', 'XT0', RK, RKk, Min), (0, BT, RT, 'XT2', 'XT0', RB, RBk, Min)]
                        for (b0, L, Rr, Lk, Rk_, dst, dk, msk) in plan:
                            reg, rks = preg(b0, 2)
                            for j in range(8):
                                mm(hs128(reg, j), hs128(L, j), hs128(Rr, j), [Lk + "_%d" % (j // 4), Rk_ + "_%d" % (j // 4)], [rks[j // 4]], r32=True)
                            for bnk in range(2):
                                bs = slice(bnk * 512, (bnk + 1) * 512)
                                tt('dve', v3(dst[:, bs], 4), v3(reg[:, bs], 4), msk.unsqueeze(1).to_broadcast([128, 4, 128]), ALU.mult,
                                   [rks[bnk], 'cst'], [dk + "_%d" % bnk if dk.startswith('xyw') else dk])

                        if rstop == 5:
                            S.barrier()
                            return nc
                        stt('dve', v3(Wa[:], 8), v3(Xa[:], 8), -1.0, ident.unsqueeze(1).to_broadcast([128, 8, 128]), ALU.mult, ALU.add,
                            [xk[0] + "_0", xk[0] + "_1", 'cst'], [xk[4] + "_0", xk[4] + "_1"])
                        Xc, Yc, Xn, Yn, Wc, Wn = Xa, Ya, Xb, Yb, Wa, Wb
                        Xck, Yck, Xnk, Ynk, Wck, Wnk = xk[0], xk[2], xk[1], xk[3], xk[4], xk[5]
                        for lev in range(6):
                            regP, kP = preg(2, 2); regQ, kQ = preg(4, 2); regR, kR = preg(6, 2)
                            for half in range(2):
                                hk_ = "_%d" % half
                                if lev < 5:
                                    for j in range(4 * half, 4 * half + 4):
                                        mm(hs128(regP, j), hs128(Yc, j), hs128(Xc, j), [Xck + hk_, Yck + hk_], [kP[half]], r32=True)
                                for j in range(4 * half, 4 * half + 4):
                                    mm(hs128(regQ, j), hs128(Xc, j), hs128(Yc, j), [Xck + hk_, Yck + hk_], [kQ[half]], r32=True)
                            for bnk in range(2):
                                bs = slice(bnk * 512, (bnk + 1) * 512)
                                hk_ = "_%d" % bnk
                                if lev < 5:
                                    act(Xn[:, bs], regP[:, bs], AF.Identity, [kP[bnk]], [Xnk + hk_])
                                cp('dve', Yn[:, bs], regQ[:, bs], [kQ[bnk]], [Ynk + hk_])
                            for j in range(8):
                                hk_ = "_%d" % (j // 4)
                                mm(hs128(regR, j), hs128(Yn, j), hs128(Wc, j), [Ynk + hk_, Wck + hk_], [kR[j // 4]], r32=True)
                            for bnk in range(2):
                                bs = slice(bnk * 512, (bnk + 1) * 512)
                                hk_ = "_%d" % bnk
                                tt('dve', Wn[:, bs], regR[:, bs], Wc[:, bs], ALU.add, [kR[bnk], Wck + hk_], [Wnk + hk_])
                            Xc, Xn, Xck, Xnk = Xn, Xc, Xnk, Xck
                            Yc, Yn, Yck, Ynk = Yn, Yc, Ynk, Yck
                            Wc, Wn, Wck, Wnk = Wn, Wc, Wnk, Wck

                        if rstop == 6:
                            S.barrier()
                            return nc
                        hsl = slice(hh * 512, (hh + 1) * 512)
                        p0, k0 = bank(0); p1, k1 = bank(1)
                        for j in range(8):
                            h = hh * 8 + j
                            o64 = p0[:, j * 64:(j + 1) * 64]
                            mm(o64, hs128(PT, j), G0[d][:, h * 64:(h + 1) * 64], ['XT3_%d' % (j // 4), G0k], [k0], start=True, stop=False)
                            mm(o64, hs128(AK, j), vr[:, h * 64:(h + 1) * 64], [AKk, 'zm'], [k0], start=False, stop=True)
                        cp('dve', Xin[:, hsl], p0, [k0], [Xink])
                        if rstop == 61:
                            S.barrier()
                            return nc
                        for j in range(8):
                            h = hh * 8 + j
                            mm(p1[:, j * 64:(j + 1) * 64], hs128(Wc, j), Xin[:, h * 64:(h + 1) * 64], [Wck + "_%d" % (j // 4), Xink], [k1])
                        if rstop == 620:
                            S.barrier()
                            return nc
                        ts('dve', Uneg[:, hsl], p1, -1.0, None, ALU.mult, None, [k1], [Unk])
                        if rstop == 62:
                            S.barrier()
                            return nc
                        if lat:
                            for j in range(8):
                                h = hh * 8 + j
                                o64 = p0[:, j * 64:(j + 1) * 64]
                                mm(o64, hs128(RT, j), G0[d][:, h * 64:(h + 1) * 64], ['XT0_%d' % (j // 4), G0k], [k0], start=True, stop=False)
                                mm(o64, hs128(RK, j), vr[:, h * 64:(h + 1) * 64], [RKk, 'zm'], [k0], start=False, stop=False)
                                mm(o64, hs128(RB, j), Uneg[:, h * 64:(h + 1) * 64], [RBk, Unk], [k0], start=False, stop=True)
                            act(o_[:, hsl], p0, AF.Identity, [k0], [ok])
                        for j in range(8):
                            h = hh * 8 + j
                            o64 = p1[0:64, j * 64:(j + 1) * 64]
                            mm(o64, Kt[:, h * 64:(h + 1) * 64], vr[:, h * 64:(h + 1) * 64], [gk[9], 'zm'], [k1], start=True, stop=False)
                            mm(o64, Bt[:, h * 64:(h + 1) * 64], Uneg[:, h * 64:(h + 1) * 64], [gk[10], Unk], [k1], start=False, stop=True)
                        tt('dve', G0[d][0:64, hsl], p1[0:64, :], G0[d][0:64, hsl], ALU.add, [k1, G0k], [G0k])
                        tt('dve', v3(G0[d][0:64, hsl], 8), v3(G0[d][0:64, hsl], 8),
                           gam[:].rearrange("p (h x) -> p h x", x=16)[:, hh * 8:(hh + 1) * 8, 0:1].to_broadcast([64, 8, 64]),
                           ALU.mult, [G0k, 'gam'], [G0k])
                        if rstop == 63:
                            S.barrier()
                            return nc
                    if rstop == 7:
                        S.barrier()
                        return nc
                    if i == 1 and d == 0:
                        dump('sr_f', G0[0][0:64, :], G0k)
                    if rstop == 8 and i == 1 and d == 0:
                        S.barrier()
                        return nc
                    if not lat:
                        continue
                    if d == 0:
                        if li == 0:
                            dump('orf_0', o_[:], ok)
                        dma('sp', orf_scr[tokrows(li), :], o_[:], reads=[ok], writes=['orf%d' % li])
                        if rstop == 9:
                            S.barrier()
                            return nc
                        if rstop == 10 and li == 15:
                            S.barrier()
                            return nc
                        continue
                    orf, ygl, zgr, tmp2 = G[7], G[8], G[9], G[10]
                    dma('sp', orf[:], orf_scr[tokrows(li), :], reads=['orf%d' % li], writes=[gk[7]])
                    dma('act', ygl[:], ygla_scr[tokrows(li), :], writes=[gk[8]])
                    dma('sp', zgr[:], z_scr[tokrows(i), GATE0 + 1024:GATE0 + 2048], writes=[gk[9]])
                    tt('dve', o_[:], o_[:], orf[:], ALU.add, [ok, gk[7]], [ok])
                    if li == 0:
                        dump('orsum_0', o_[:], ok)
                    red(s16[:], v3(o_[:], 16), [ok], ['s16'])
                    ts('dve', s16[:], s16[:], 1.0 / 64, None, ALU.mult, None, ['s16'], ['s16'])
                    tt('dve', v3(o_[:], 16), v3(o_[:], 16), s16[:].unsqueeze(2).to_broadcast([128, 16, 64]), ALU.subtract, [ok, 's16'], [ok])
                    tt('dve', tmp2[:], o_[:], o_[:], ALU.mult, [ok], [gk[10]])
                    red(s16[:], v3(tmp2[:], 16), [gk[10]], ['s16'])
                    rstd_from(s16[:], 64, 64e-5, 's16')
                    tt('dve', v3(o_[:], 16), v3(o_[:], 16), s16[:].unsqueeze(2).to_broadcast([128, 16, 64]), ALU.mult, [ok, 's16'], [ok])
                    tt('dve', o_[:], o_[:], bcs['lnw'][:], ALU.mult, [ok, 'bc_lnw'], [ok])
                    tt('dve', o_[:], o_[:], bcs['lnb'][:], ALU.add, [ok, 'bc_lnb'], [ok])
                    tt('dve', tmp2[:], r_, km_[:], ALU.mult, ['zm', gk[3]], [gk[10]])
                    tt('dve', tmp2[:], tmp2[:], bcs['rk'][:], ALU.mult, [gk[10], 'bc_rk'], [gk[10]])
                    red(s16[:], v3(tmp2[:], 16), [gk[10]], ['s16'])
                    tt('dve', v3(tmp2[:], 16), v3(vr, 16), s16[:].unsqueeze(2).to_broadcast([128, 16, 64]), ALU.mult, ['zm', 's16'], [gk[10]])
                    tt('dve', o_[:], o_[:], tmp2[:], ALU.add, [ok, gk[10]], [ok])
                    act(sg[:], zm[:, 3264:3424], AF.Sigmoid, ['zm'], ['sgl'])
                    p2, k2 = bank(2)
                    tr(p2[:, 0:128], sg[:, 0:128], ['sgl'], [k2])
                    tr(p2[0:32, 128:256], sg[:, 128:160], ['sgl'], [k2])
                    cp('dve', gT0[:], p2[:, 0:128], [k2], ['gT0'])
                    cp('dve', gT1[:], p2[0:32, 128:256], [k2], ['gT1'])
                    for hf in range(2):
                        pb, pk = bank(3 + hf)
                        hs = slice(hf * 512, (hf + 1) * 512)
                        mm(pb, gT0[:], G2a[:, hs], ['gT0', 'G2a'], [pk], start=True, stop=False)
                        mm(pb, gT1[:], G2b[:, hs], ['gT1', 'G2b'], [pk], start=False, stop=True)
                        tt('dve', o_[:, hs], o_[:, hs], pb, ALU.mult, [ok, pk], [ok])
                    act(zgr[:], zgr[:], AF.Sigmoid, [gk[9]], [gk[9]])
                    tt('dve', o_[:], o_[:], zgr[:], ALU.mult, [ok, gk[9]], [ok])
                    tt('dve', o_[:], o_[:], ygl[:], ALU.add, [ok, gk[8]], [ok])
                    dma('sp', ymix_scr[tokrows(li), :], o_[:], reads=[ok], writes=['ymix%d' % li])
                    if rstop == 11:
                        S.barrier()
                        return nc
            convert_chunks(128)
        S.barrier()
        if 'ymix' in dbg_d:
            for i in range(16):
                dma('sp', dbg_d['ymix'][tokrows(i), :], ymix_scr[tokrows(i), :])
            S.barrier()
        if stop == 'p4':
            return nc

        with ExitStack() as st:
            Wo = sbt(st, "Wo", [128, 8, 1024], BF16); Wq = sbt(st, "Wq", [128, 8, 2048], BF16)
            SKT = sbt(st, "SKT", [128, 16, 128], BF16)
            with ExitStack() as st0:
                stg = [sbt(st0, "stg%d" % j, [128, 8, 512]) for j in range(2)]
                n = 0
                for (src, dst, nblk) in [(wout_d, Wo, 2), (wq_d, Wq, 4)]:
                    for blk in range(nblk):
                        sgb = stg[n % 2]; sgk = "stg%d" % (n % 2)
                        dma('sp' if n % 2 == 0 else 'act', sgb[:], src[:, blk * 512:(blk + 1) * 512].rearrange("(k p) n -> p k n", p=128), writes=[sgk])
                        cp('dve', dst[:, :, blk * 512:(blk + 1) * 512], sgb[:], [sgk], ['W' + str(n)])
                        n += 1
                skf = sbt(st0, "skf", [128, 16, 128])
                dma('sp', skf[:], sk_d.rearrange("b n d -> n b d"), writes=['skf'])
                for blk in range(16):
                    pb, pk = bank(blk // 4)
                    tr(pb[:, (blk % 4) * 128:(blk % 4 + 1) * 128], skf[:, blk, :], ['skf'], [pk])
                for bnk in range(4):
                    cp('dve', SKT[:, bnk * 4:(bnk + 1) * 4, :].rearrange("p b n -> p (b n)"), bank(bnk)[0], [bank(bnk)[1]], ['SKT'])
                S.barrier()
            bc2 = {}
            for nm, src in [('m2', m_scr[0, 2 * D:3 * D]), ('sh2', m_scr[0, 3 * D:4 * D]), ('g2', m_scr[0, 4 * D:5 * D]),
                            ('m5', m_scr[0, 5 * D:6 * D]), ('fnw', fnw_d)]:
                bc2[nm] = sbt(st, "b2_" + nm, [128, D])
                dma('sp', bc2[nm][:], src.partition_broadcast(128), writes=['b2_' + nm])
            with ExitStack() as stn:
                n2wt = sbt(stn, "b2_n2w", [128, D])
                dma('sp', n2wt[:], n2w_d.partition_broadcast(128), writes=['b2_n2w'])
                stt('dve', bc2['g2'][:], bc2['g2'][:], 1.0, n2wt[:], ALU.add, ALU.mult, ['b2_g2', 'b2_n2w'], ['b2_g2'])
                S.barrier()
            ym = sbt(st, "ym", [128, D]); yT = sbt(st, "yT", [128, 8, 128], BF16)
            h1s = [sbt(st, "h1_%d" % j, [128, D]) for j in range(2)]; a2s = [sbt(st, "a2_%d" % j, [128, D]) for j in range(2)]; a2T = sbt(st, "a2T", [128, 8, 128], BF16)
            qT = sbt(st, "qT", [128, 16, 128], BF16); s_sb = sbt(st, "s_sb", [128, 2048]); s2 = sbt(st, "s2", [128, 2048])
            vals = sbt(st, "vals", [128, 16, 16]); idxs = sbt(st, "idxs", [128, 16, 16], U32); idxf = sbt(st, "idxf", [128, 16, 16])
            cand3 = s_sb[:].rearrange("p (h c) -> p h c", h=8); cand23 = s2[:].rearrange("p (h c) -> p h c", h=8)
            tops = sbt(st, "tops", [128, 8, 16]); pos = sbt(st, "pos", [128, 8, 16], U32); posf = sbt(st, "posf", [128, 8, 16])
            pj = sbt(st, "pj", [128, 8, 16]); pi_ = sbt(st, "pi", [128, 8, 16]); sel1 = sbt(st, "sel1", [128, 8, 16]); sel2 = sbt(st, "sel2", [128, 8, 16])
            idx_is = [sbt(st, "idx_i%d" % j, [128, 128], I32) for j in range(2)]; gatess = [sbt(st, "gates%d" % j, [128, 8, 16]) for j in range(2)]; g8 = sbt(st, "gsum8", [128, 8])
            zsc = sbt(st, "zsc", [128, 128]); Aw = sbt(st, "Aw", [128, 128])
            GS = 4
            NGB = 14
            UV = [sbt(st, "UV%d" % j, [128, 2 * D], BF16) for j in range(NGB)]
            prods = [sbt(st, "prod%d" % j, [128, D]) for j in range(2)]
            junk = sbt(st, "junk6", [128, D], BF16); acc = sbt(st, "acc", [128, D])
            identb = sbt(st, "identb", [128, 128], BF16)
            cp('dve', identb[:], ident, ['cst'], ['identb'])
            dg = [sbt(st, "dg%d" % j, [128, GS, 128], BF16) for j in range(2)]
            ss6 = sbt(st, "ss6", [128, 2])
            thr16 = sbt(st, "thr16", [128, 16])
            ts('dve', thr16[:], iota16, 16.0, 16.0, ALU.mult, ALU.add, ['cst'], ['thr16'])
            def front(li):
                fb = li % 2
                dma('sp', ym[:], ymix_scr[tokrows(li), :], writes=['ym'])
                dma('act', h1s[fb][:], x_d[tokrows(li), :], writes=['h1_%d' % fb])
                for k in range(8):
                    pb, pk = bank(k // 4)
                    tr(pb[:, (k % 4) * 128:(k % 4 + 1) * 128], ym[:, k * 128:(k + 1) * 128], ['ym'], [pk])
                for bnk in range(2):
                    cp('dve', yT[:, bnk * 4:(bnk + 1) * 4, :].rearrange("p k t -> p (k t)"), bank(bnk)[0], [bank(bnk)[1]], ['yT'])
                for hf in range(2):
                    pb, pk = bank(2 + hf)
                    hs = slice(hf * 512, (hf + 1) * 512)
                    for k in range(8):
                        mm(pb, yT[:, k, :], Wo[:, k, hs], ['yT', 'W0', 'W1'], [pk], start=(k == 0), stop=(k == 7))
                    tt('dve', a2s[fb][:, hs], pb, bc2['m2'][:, hs], ALU.mult, [pk, 'b2_m2'], ['a2_%d' % fb])
                tt('dve', h1s[fb][:], h1s[fb][:], a2s[fb][:], ALU.add, ['h1_%d' % fb, 'a2_%d' % fb], ['h1_%d' % fb])
                yield
                if li == 0:
                    dump('h1_0', h1s[fb][:], 'h1_%d' % fb)
                act(junk[:], h1s[fb][:], AF.Square, ['h1_%d' % fb], ['junk6', 'ss6a'], accum_out=ss6[:, 0:1])
                rstd_from(ss6[:, 0:1], D, 1e-6, 'ss6a')
                ts('dve', a2s[fb][:], h1s[fb][:], ss6[:, 0:1], None, ALU.mult, None, ['h1_%d' % fb, 'ss6a'], ['a2_%d' % fb])
                tt('dve', a2s[fb][:], a2s[fb][:], bc2['g2'][:], ALU.mult, ['a2_%d' % fb, 'b2_g2'], ['a2_%d' % fb])
                tt('dve', a2s[fb][:], a2s[fb][:], bc2['sh2'][:], ALU.add, ['a2_%d' % fb, 'b2_sh2'], ['a2_%d' % fb])
                for k in range(8):
                    pb, pk = bank(k // 4)
                    tr(pb[:, (k % 4) * 128:(k % 4 + 1) * 128], a2s[fb][:, k * 128:(k + 1) * 128], ['a2_%d' % fb], [pk])
                for bnk in range(2):
                    cp('dve', a2T[:, bnk * 4:(bnk + 1) * 4, :].rearrange("p k t -> p (k t)"), bank(bnk)[0], [bank(bnk)[1]], ['a2T'])
                for blk in range(16):
                    pb, pk = bank(2 + blk // 4)
                    for k in range(8):
                        mm(pb[:, (blk % 4) * 128:(blk % 4 + 1) * 128], Wq[:, k, blk * 128:(blk + 1) * 128], a2T[:, k, :],
                           ['a2T', 'W2', 'W3', 'W4', 'W5'], [pk], start=(k == 0), stop=(k == 7))
                for bnk in range(4):
                    act(qT[:, bnk * 4:(bnk + 1) * 4, :].rearrange("p b t -> p (b t)"), bank(2 + bnk)[0], AF.Identity, [bank(2 + bnk)[1]], ['qT'])
                for blk in range(16):
                    pb, pk = bank(blk // 4)
                    mm(pb[:, (blk % 4) * 128:(blk % 4 + 1) * 128], qT[:, blk, :], SKT[:, blk, :], ['qT', 'SKT'], [pk])
                for bnk in range(4):
                    cp('dve', s_sb[:, bnk * 512:(bnk + 1) * 512], bank(bnk)[0], [bank(bnk)[1]], ['s_sb'])
                for blk in range(16):
                    sv = s_sb[:, blk * 128:(blk + 1) * 128]; s2v = s2[:, blk * 128:(blk + 1) * 128]
                    op('dve', lambda e: e.max(out=vals[:, blk, 0:8], in_=sv), reads=['s_sb'], writes=['vals'])
                    op('dve', lambda e: e.max_index(out=idxs[:, blk, 0:8], in_max=vals[:, blk, 0:8], in_values=sv), reads=['s_sb', 'vals'], writes=['idxs'])
                    op('dve', lambda e: e.match_replace(out=s2v, in_to_replace=vals[:, blk, 0:8], in_values=sv, imm_value=-1e30), reads=['s_sb', 'vals'], writes=['s2'])
                    op('dve', lambda e: e.max(out=vals[:, blk, 8:16], in_=s2v), reads=['s2'], writes=['vals'])
                    op('dve', lambda e: e.max_index(out=idxs[:, blk, 8:16], in_max=vals[:, blk, 8:16], in_values=s2v), reads=['s2', 'vals'], writes=['idxs'])
                    yield
                v4 = vals[:].rearrange("p (h two) s -> p h two s", two=2)
                tt('dve', cand3.rearrange("p h (i j) -> p h i j", i=16), v4[:, :, 0, :].unsqueeze(3).to_broadcast([128, 8, 16, 16]),
                   v4[:, :, 1, :].unsqueeze(2).to_broadcast([128, 8, 16, 16]), ALU.add, ['vals'], ['s_sb'])
                for h in range(8):
                    cv = cand3[:, h, :]; c2v = cand23[:, h, :]
                    op('dve', lambda e: e.max(out=tops[:, h, 0:8], in_=cv), reads=['s_sb'], writes=['tops'])
                    op('dve', lambda e: e.max_index(out=pos[:, h, 0:8], in_max=tops[:, h, 0:8], in_values=cv), reads=['s_sb', 'tops'], writes=['pos'])
                    op('dve', lambda e: e.match_replace(out=c2v, in_to_replace=tops[:, h, 0:8], in_values=cv, imm_value=-1e30), reads=['s_sb', 'tops'], writes=['s2'])
                    op('dve', lambda e: e.max(out=tops[:, h, 8:16], in_=c2v), reads=['s2'], writes=['tops'])
                    op('dve', lambda e: e.max_index(out=pos[:, h, 8:16], in_max=tops[:, h, 8:16], in_values=c2v), reads=['s2', 'tops'], writes=['pos'])
                    yield
                cp('dve', posf[:], pos[:], ['pos'], ['posf'])
                yield
                cp('dve', idxf[:], idxs[:], ['idxs'], ['idxf'])
                tt('dve', s2[:].rearrange("p (h a b) -> p h a b", h=8, a=16), posf[:].unsqueeze(3).to_broadcast([128, 8, 16, 16]),
                   thr16[:].unsqueeze(1).unsqueeze(1).to_broadcast([128, 8, 16, 16]), ALU.is_ge, ['posf', 'thr16'], ['s2'])
                red(pi_[:].rearrange("p h s -> p (h s)"), s2[:].rearrange("p (x b) -> p x b", b=16), ['s2'], ['pi'])
                stt('dve', pj[:], pi_[:], -16.0, posf[:], ALU.mult, ALU.add, ['pi', 'posf'], ['pj'])
                i4 = idxf[:].rearrange("p (h two) s -> p h two s", two=2)
                oh = s2[:].rearrange("p (h a b) -> p h a b", h=8, a=16)
                pr4 = s_sb[:].rearrange("p (h a b) -> p h a b", h=8, a=16)
                for (pp, which, selt, selk) in [(pi_, 0, sel1, 'sel1'), (pj, 1, sel2, 'sel2')]:
                    tt('dve', oh, pp[:].unsqueeze(3).to_broadcast([128, 8, 16, 16]),
                       iota16.unsqueeze(1).unsqueeze(1).to_broadcast([128, 8, 16, 16]), ALU.is_equal, ['pi', 'pj', 'cst'], ['s2'])
                    tt('dve', pr4, oh, i4[:, :, which, :].unsqueeze(2).to_broadcast([128, 8, 16, 16]), ALU.mult, ['s2', 'idxf'], ['s_sb'])
                    red(selt[:].rearrange("p h s -> p (h s)"), s_sb[:].rearrange("p (x b) -> p x b", b=16), ['s_sb'], [selk])
                stt('dve', sel1[:], sel1[:], 128.0, sel2[:], ALU.mult, ALU.add, ['sel1', 'sel2'], ['sel1'])
                yield
                ts('dve', sel1[:], sel1[:], 0.0, float(NE - 1), ALU.max, ALU.min, ['sel1'], ['sel1'])
                cp('dve', idx_is[fb][:], sel1[:].rearrange("p h s -> p (h s)"), ['sel1'], ['idx_i%d' % fb])
                tt('dve', gatess[fb][:], tops[:], tops[:, :, 0:1].to_broadcast([128, 8, 16]), ALU.subtract, ['tops'], ['gates%d' % fb])
                act(gatess[fb][:], gatess[fb][:], AF.Exp, ['gates%d' % fb], ['gates%d' % fb])
                red(g8[:], gatess[fb][:], ['gates%d' % fb], ['g8'])
                op('dve', lambda e: e.reciprocal(out=g8[:], in_=g8[:]), reads=['g8'], writes=['g8'])
                tt('dve', gatess[fb][:], gatess[fb][:], g8[:].unsqueeze(2).to_broadcast([128, 8, 16]), ALU.mult, ['gates%d' % fb, 'g8'], ['gates%d' % fb])
                if li == 0:
                    dump('idx_0', sel1[:].rearrange("p h s -> p (h s)"), 'sel1')
                    dump('gates_0', gatess[fb][:].rearrange("p h s -> p (h s)"), 'gates%d' % fb)
                yield
            g0 = front(0)
            for _ in g0:
                pass
            for li in range(16):
                nxt = front(li + 1) if li + 1 < 16 else None
                g2d = gatess[li % 2][:].rearrange("p h s -> p (h s)")

                def vacc(g):
                    d_ = dg[g % 2]; dk = "dg%d" % (g % 2)
                    tt('dve', d_[:], identb[:].unsqueeze(1).to_broadcast([128, GS, 128]),
                       Aw[:, g * GS:(g + 1) * GS].unsqueeze(2).to_broadcast([128, GS, 128]), ALU.mult,
                       ['identb', 'Aw%d' % (g % 3)], [dk])
                    for q in range(GS):
                        s_ = g * GS + q
                        bk = "UV%d" % (s_ % NGB)
                        for hf in range(2):
                            pb, pk = bank(6 + hf)
                            mm(pb, d_[:, q, :], UV[s_ % NGB][:, D + hf * 512:D + (hf + 1) * 512], [dk, bk], [pk],
                               start=(s_ == 0), stop=(s_ == 127))
                ngrp = 128 // GS
                for g in range(ngrp):
                    gsl = slice(g * GS, (g + 1) * GS)
                    for q in range(GS):
                        s_ = g * GS + q
                        b_uv = UV[s_ % NGB]; bk = "UV%d" % (s_ % NGB)
                        dma('pool', b_uv[:], uv16_scr, reads=['idx_i%d' % (li % 2)], writes=[bk],
                            indirect=bass.IndirectOffsetOnAxis(ap=idx_is[li % 2][:, s_:s_ + 1], axis=0))
                        pr = prods[s_ % 2]; prk = "prod%d" % (s_ % 2)
                        tt('dve', pr[:], b_uv[:, 0:D], a2s[li % 2][:], ALU.mult, [bk, 'a2_%d' % (li % 2)], [prk])
                        act(junk[:], pr[:], AF.Identity, [prk], ['junk6', 'zsc%d' % (g % 3)], accum_out=zsc[:, s_:s_ + 1])
                    if g > 0:
                        vacc(g - 1)
                    act(Aw[:, gsl], zsc[:, gsl], AF.Gelu, ['zsc%d' % (g % 3)], ['Aw%d' % (g % 3)])
                    tt('dve', Aw[:, gsl], Aw[:, gsl], g2d[:, gsl], ALU.mult, ['Aw%d' % (g % 3), 'gates%d' % (li % 2)], ['Aw%d' % (g % 3)])
                    if nxt is not None:
                        for _ in range(2):
                            next(nxt, None)
                vacc(ngrp - 1)
                cp('dve', acc[:, 0:512], bank(6)[0], ['ps6'], ['acc'])
                cp('dve', acc[:, 512:1024], bank(7)[0], ['ps7'], ['acc'])
                if li == 0:
                    dump('f_0', acc[:], 'acc')
                if nxt is not None:
                    for _ in nxt:
                        pass
                tt('dve', acc[:], acc[:], bc2['m5'][:], ALU.mult, ['acc', 'b2_m5'], ['acc'])
                tt('dve', acc[:], acc[:], h1s[li % 2][:], ALU.add, ['acc', 'h1_%d' % (li % 2)], ['acc'])
                act(junk[:], acc[:], AF.Square, ['acc'], ['junk6', 'ss6b'], accum_out=ss6[:, 1:2])
                rstd_from(ss6[:, 1:2], D, 1e-6, 'ss6b')
                ts('dve', acc[:], acc[:], ss6[:, 1:2], None, ALU.mult, None, ['acc', 'ss6b'], ['acc'])
                tt('dve', acc[:], acc[:], bc2['fnw'][:], ALU.mult, ['acc', 'b2_fnw'], ['acc'])
                dma('sp', out_d[tokrows(li), :], acc[:], reads=['acc'])
        S.barrier()
    return nc


_CACHE = {}


def _consts():
    p = np.arange(128)[:, None]; f = np.arange(128)[None, :]
    le = (p <= f).astype(np.float32); lt = (p < f).astype(np.float32)
    ge = (p >= f).astype(np.float32); gt = (p > f).astype(np.float32)
    shm = np.zeros((128, 16), np.float32)
    pp = np.arange(128)
    shm[:, 0] = (pp % 64 != 0); shm[:, 1] = (pp % 64 != 63); shm[:, 2] = (pp >= 64); shm[:, 3] = (pp < 64)
    shm[:, 4] = 1.0; shm[:, 5] = (pp != 0); shm[:, 6] = (pp != 127); shm[:, 7] = -C0; shm[:, 8] = -C0
    iota = np.tile(np.arange(16, dtype=np.float32)[None, :], (128, 1))
    return np.concatenate([np.eye(128, dtype=np.float32), le, lt, ge, gt, le * (-1.0 / 16), ge * (-1.0 / 16),
                           le * (-C0), ge * (-C0), shm, iota], axis=1).astype(np.float32)


def make_in_maps(inp, cores):
    f = lambda a: np.ascontiguousarray(np.asarray(a, dtype=np.float32))
    wa = np.zeros((33, 1024), np.float32)
    wa[0:16, 0:512] = inp['gla_w_a2'][0, 0]; wa[16:32, 512:1024] = inp['gla_w_a2'][0, 1]
    wa[32, 0:512] = inp['gla_b_a'][0, 0]; wa[32, 512:1024] = inp['gla_b_a'][0, 1]
    w2a = np.concatenate([inp['rwkv_w2'][0], inp['rwkv_w0'][0][:, None, :]], axis=1)
    a2a = np.concatenate([inp['rwkv_a2'][0], inp['rwkv_a0'][0][None, :]], axis=0)
    shared = dict(
        norm1_w=f(inp['norm1_w'][0]), w_mod=f(inp['w_mod'][0]), b_mod=f(inp['b_mod'][0]), w_in=f(inp['w_in'][0]),
        gla_wa=f(wa), gla_norm_w=f(inp['gla_norm_w'][0]), rwkv_mu=f(inp['rwkv_mu'][0]), rwkv_w2a=f(w2a), rwkv_a2a=f(a2a),
        rwkv_g2=f(inp['rwkv_g2'][0]), rwkv_k_k=f(inp['rwkv_k_k'][0]), rwkv_k_a=f(inp['rwkv_k_a'][0]),
        rwkv_r_k=f(inp['rwkv_r_k'][0].reshape(-1)), rwkv_ln_w=f(inp['rwkv_ln_w'][0]), rwkv_ln_b=f(inp['rwkv_ln_b'][0]),
        w_out=f(inp['w_out'][0]), norm2_w=f(inp['norm2_w'][0]), peer_w_q=f(inp['peer_w_q'][0]),
        peer_sub_keys=f(inp['peer_sub_keys'][0].reshape(16, 128, 128)), peer_u=f(inp['peer_u'][0]), peer_v=f(inp['peer_v'][0]),
        final_norm_w=f(inp['final_norm_w']), cst=_consts())
    maps = []
    for b in cores:
        m = dict(shared)
        m['x'] = f(inp['x'][b]); m['ctx'] = f(inp['ctx'][b])
        m['c2'] = f(np.stack([inp['c'][b], inp['c_ctx']], axis=0))
        maps.append(m)
    return maps


def kernel(**inputs):
    if 'nc' not in _CACHE:
        _CACHE['nc'] = build_nc()
    nc = _CACHE['nc']
    maps = make_in_maps(inputs, list(range(8)))
    res = run_bass_kernel_spmd(nc, maps, core_ids=list(range(8)))
    return np.stack([np.asarray(r['out'], dtype=np.float32) for r in res.results], axis=0)
```

```python
import numpy as np
from contextlib import ExitStack
import concourse.bass as bass
import concourse.mybir as mybir
from concourse.bass_utils import run_bass_kernel_spmd

F32 = mybir.dt.float32
BF16 = mybir.dt.bfloat16
F32R = mybir.dt.float32r
FAST32 = True
I32 = mybir.dt.int32
U32 = mybir.dt.uint32
ALU = mybir.AluOpType
AF = mybir.ActivationFunctionType
AX = mybir.AxisListType

D = 1024
SEQ = 2048
CTX = 256
NTOK = SEQ + CTX
NT = NTOK // 128
INC = 8576
GLA0, RW0, GATE0 = 0, 3104, 6528
RWC = 3424
C0 = 0.6065306597126334
NE = 16384


class Sched:
    LIM = 3500
    DLIM = 3072

    def __init__(self, nc, stack, ndma=16):
        self.nc = nc
        self.stack = stack
        self.eng = dict(pe=nc.tensor, dve=nc.vector, act=nc.scalar, pool=nc.gpsimd, sp=nc.sync)
        self.semobj = {}
        self.nsem = 0
        self.cur = {}
        self.cnt = {}
        self.ep = {}
        for e in self.eng:
            self.ep[e] = 0
            self.cur[e] = (e, 0)
            self.semobj[self.cur[e]] = self._newsem()
            self.cnt[e] = 0
        self.nhw = ndma
        self.nsw = 10
        ndma = self.nhw + self.nsw
        self.ndma = ndma
        self.dcur = []
        self.dval = [0] * ndma
        self.dep = [0] * ndma
        for i in range(ndma):
            k = ('d', i, 0)
            self.semobj[k] = self._newsem()
            self.dcur.append(k)
        self.dpend = {}
        self.dnext = 0
        self.dnext_sw = 0
        self.seen = {e: {} for e in self.eng}
        self.state = {}
        self.ninst = 0

    def _newsem(self):
        self.nsem += 1
        return self.stack.enter_context(self.nc.semaphore("sm%d" % self.nsem))

    def _deps(self, reads, writes):
        deps = {}

        def add(ev):
            if ev is not None and deps.get(ev[0], 0) < ev[1]:
                deps[ev[0]] = ev[1]
        for key in reads:
            st = self.state.get(key)
            if st is not None:
                add(st[0])
        for key in writes:
            st = self.state.get(key)
            if st is not None:
                add(st[0])
                for k, v in st[1].items():
                    add((k, v))
        return deps

    def _wait(self, eng, deps):
        seen = self.seen[eng]
        for k, v in deps.items():
            if k[0] == 'pe' and eng == 'pe':
                continue
            if seen.get(k, 0) >= v:
                continue
            self.eng[eng].wait_ge(self.semobj[k], v)
            self.ninst += 1
            seen[k] = v

    def _record(self, ev, reads, writes):
        for key in reads:
            st = self.state.setdefault(key, [None, {}])
            if st[1].get(ev[0], 0) < ev[1]:
                st[1][ev[0]] = ev[1]
        for key in writes:
            self.state[key] = [ev, {}]

    def op(self, eng, fn, reads=(), writes=()):
        psr = [k for k in reads if isinstance(k, str) and k.startswith('ps') and k[2:].isdigit()]
        if psr:
            reads = [k for k in reads if k not in psr]
            writes = list(writes) + [k for k in psr if k not in writes]
        self._wait(eng, self._deps(reads, writes))
        ins = fn(self.eng[eng])
        self.cnt[eng] += 1
        k = self.cur[eng]
        ins.then_inc(self.semobj[k], 1)
        self.ninst += 1
        self._record((k, self.cnt[eng]), reads, writes)
        if self.cnt[eng] >= self.LIM:
            self.ep[eng] += 1
            self.cur[eng] = (eng, self.ep[eng])
            self.semobj[self.cur[eng]] = self._newsem()
            self.cnt[eng] = 0
            self.prev_last = getattr(self, 'prev_last', {})
            self.prev_last[eng] = (k, self.LIM)
        return ins

    def dma(self, q, out, in_, reads=(), writes=(), indirect=None, **kw):
        deps = self._deps(reads, writes)
        if q == 'pool':
            i = self.nhw + self.dnext_sw
            self.dnext_sw = (self.dnext_sw + 1) % self.nsw
        else:
            i = self.dnext
            self.dnext = (self.dnext + 1) % self.nhw
        if self.dval[i] >= self.DLIM:
            self.dpend[self.dcur[i]] = self.dval[i]
            self.dep[i] += 1
            self.dcur[i] = ('d', i, self.dep[i])
            self.semobj[self.dcur[i]] = self._newsem()
            self.dval[i] = 0
        k = self.dcur[i]
        if self.dval[i] > 0 and deps.get(k, 0) < self.dval[i]:
            deps[k] = self.dval[i]
        self._wait(q, deps)
        e = self.eng[q]
        if indirect is None:
            ins = e.dma_start(out=out, in_=in_, **kw)
        else:
            ins = e.indirect_dma_start(out=out, out_offset=None, in_=in_, in_offset=indirect)
        self.dval[i] += 16
        ins.then_inc(self.semobj[k], 16)
        self.ninst += 1
        self._record((k, self.dval[i]), reads, writes)
        return ins

    def barrier(self):
        for e in self.eng:
            deps = {}
            for f in self.eng:
                if f == e and e == 'pe':
                    continue
                if self.cnt[f] > 0:
                    deps[self.cur[f]] = self.cnt[f]
                elif self.ep[f] > 0:
                    deps[(f, self.ep[f] - 1)] = self.LIM
            for i in range(self.ndma):
                if self.dval[i] > 0:
                    deps[self.dcur[i]] = self.dval[i]
            for k, v in self.dpend.items():
                deps[k] = v
            self._wait(e, deps)
        self.dpend = {}


def build_nc(dbg=(), stop=None, rstop=0):
    nc = bass.Bass("TRN2", target_bir_lowering=False)

    def din(name, shape, dt=F32):
        return nc.dram_tensor(name, list(shape), dt, kind="ExternalInput").ap()

    def dscr(name, shape, dt=F32):
        return nc.dram_tensor(name, list(shape), dt, kind="Internal").ap()

    x_d = din("x", [SEQ, D]); ctx_d = din("ctx", [CTX, D]); c2_d = din("c2", [2, D])
    n1w_d = din("norm1_w", [D]); wmod_d = din("w_mod", [D, 6 * D]); bmod_d = din("b_mod", [6 * D])
    win_d = din("w_in", [D, INC]); wa_d = din("gla_wa", [33, 1024]); gnw_d = din("gla_norm_w", [D])
    mu_d = din("rwkv_mu", [RWC]); w2a_d = din("rwkv_w2a", [2, 65, D]); a2a_d = din("rwkv_a2a", [65, D])
    g2_d = din("rwkv_g2", [160, D]); kk_d = din("rwkv_k_k", [D]); ka_d = din("rwkv_k_a", [D])
    rk_d = din("rwkv_r_k", [D]); lnw_d = din("rwkv_ln_w", [D]); lnb_d = din("rwkv_ln_b", [D])
    wout_d = din("w_out", [D, D]); n2w_d = din("norm2_w", [D]); wq_d = din("peer_w_q", [D, 2048])
    sk_d = din("peer_sub_keys", [16, 128, 128]); pu_d = din("peer_u", [NE, D]); pv_d = din("peer_v", [NE, D])
    fnw_d = din("final_norm_w", [D])
    cst_d = din("cst", [128, 1184])
    out_d = nc.dram_tensor("out", [SEQ, D], F32, kind="ExternalOutput").ap()
    dbg_d = {}
    for name, shape in dbg:
        dbg_d[name] = nc.dram_tensor("dbg_" + name, list(shape), F32, kind="ExternalOutput").ap()

    m_scr = dscr("m_scr", [2, 6 * D])
    z_scr = dscr("z_scr", [NTOK, INC])
    ogf_scr = dscr("ogf_scr", [SEQ, D])
    orf_scr = dscr("orf_scr", [SEQ, D])
    ygla_scr = dscr("ygla_scr", [SEQ, D])
    ymix_scr = dscr("ymix_scr", [SEQ, D])
    zm_scr = dscr("zm_scr", [NTOK, RWC])
    uv16_scr = dscr("uv16_scr", [NE, 2 * D], BF16)

    with ExitStack() as top:
        S = Sched(nc, top)
        op, dma = S.op, S.dma

        def sbt(st, name, shape, dt=F32):
            return st.enter_context(nc.sbuf_tensor(name, list(shape), dt))

        psA = top.enter_context(nc.psum_tensor("psA", [128, 2048], F32))
        psB = top.enter_context(nc.psum_tensor("psB", [128, 2048], F32))

        def bank(i):
            t = psA if i < 4 else psB
            j = i % 4
            return t[:, j * 512:(j + 1) * 512], "ps%d" % i

        def pkeys(lo, n):
            return ["ps%d" % i for i in range(lo, lo + n)]

        cst = sbt(top, "cst_sb", [128, 1184])
        dma('sp', cst[:], cst_d, writes=['cst'])
        ident = cst[:, 0:128]
        M_le = cst[:, 128:256]; M_lt = cst[:, 256:384]; M_ge = cst[:, 384:512]; M_gt = cst[:, 512:640]
        gcum = [cst[:, 640:768], cst[:, 768:896]]
        rcum = [cst[:, 896:1024], cst[:, 1024:1152]]
        shm = cst[:, 1152:1168]
        iota16 = cst[:, 1168:1184]

        def mm(out, lhsT, rhs, R, W, start=True, stop=True, r32=False):
            if r32 and FAST32:
                if lhsT.dtype != F32R:
                    lhsT = lhsT.bitcast(F32R)
                if rhs.dtype != F32R:
                    rhs = rhs.bitcast(F32R)
            else:
                if lhsT.dtype == F32R:
                    lhsT = lhsT.bitcast(F32)
                if rhs.dtype == F32R:
                    rhs = rhs.bitcast(F32)
            op('pe', lambda e: e.matmul(out=out, lhsT=lhsT, rhs=rhs, start=start, stop=stop),
               reads=list(R) + ['cst'], writes=W)

        def tr(out, in_, R, W, n=128):
            op('pe', lambda e: e.transpose(out=out, in_=in_, identity=ident[0:n, 0:n]), reads=list(R) + ['cst'], writes=W)

        def act(out, in_, func, R, W, **kw):
            op('act', lambda e: e.activation(out=out, in_=in_, func=func, **kw), reads=R, writes=W)

        def tt(eng, out, in0, in1, o, R, W):
            op(eng, lambda e: e.tensor_tensor(out=out, in0=in0, in1=in1, op=o), reads=R, writes=W)

        def ts(eng, out, in0, s1, s2, o0, o1, R, W):
            if s2 is None:
                op(eng, lambda e: e.tensor_scalar(out=out, in0=in0, scalar1=s1, scalar2=None, op0=o0), reads=R, writes=W)
            else:
                op(eng, lambda e: e.tensor_scalar(out=out, in0=in0, scalar1=s1, scalar2=s2, op0=o0, op1=o1), reads=R, writes=W)

        def stt(eng, out, in0, sc, in1, o0, o1, R, W):
            op(eng, lambda e: e.scalar_tensor_tensor(out=out, in0=in0, scalar=sc, in1=in1, op0=o0, op1=o1), reads=R, writes=W)

        def cp(eng, out, in_, R, W):
            op(eng, lambda e: e.tensor_copy(out=out, in_=in_), reads=R, writes=W)

        def red(out, in_, R, W):
            op('dve', lambda e: e.tensor_reduce(out=out, in_=in_, axis=AX.X, op=ALU.add), reads=R, writes=W)

        def rstd_from(ssum, n, eps, key):
            ts('dve', ssum, ssum, 1.0 / n, eps, ALU.mult, ALU.add, [key], [key])
            act(ssum, ssum, AF.Sqrt, [key], [key])
            op('dve', lambda e: e.reciprocal(out=ssum, in_=ssum), reads=[key], writes=[key])

        def dump(name, ap, key):
            if name in dbg_d:
                dma('sp', dbg_d[name], ap, reads=[key])

        def tokrows(i):
            return slice(i * 128, (i + 1) * 128)

        with ExitStack() as st:
            c1 = sbt(st, "c1", [128, 8, 2]); sc = sbt(st, "sc", [128, 8, 2])
            bm = sbt(st, "bm", [2, 6 * D]); mrow = sbt(st, "mrow", [2, 6 * D])
            wb = [sbt(st, "wmb%d" % i, [128, 8, 512]) for i in range(2)]
            for j in range(2):
                dma('sp', c1[:, :, j], c2_d[j, :].rearrange("(k p) -> p k", p=128), writes=['c1'], allow_slow_non_contiguous=True)
            dma('sp', bm[:], bmod_d.partition_broadcast(2), writes=['bm'])
            act(sc[:], c1[:], AF.Silu, ['c1'], ['sc'])
            for nb in range(12):
                w = wb[nb % 2]; wk = "wmb%d" % (nb % 2)
                dma('sp' if nb % 2 == 0 else 'act', w[:], wmod_d[:, nb * 512:(nb + 1) * 512].rearrange("(k p) n -> p k n", p=128), writes=[wk])
                pb, pk = bank(nb % 2)
                for k in range(8):
                    mm(pb[0:2, :], sc[:, k, :], w[:, k, :], ['sc', wk], [pk], start=(k == 0), stop=(k == 7))
                tt('dve', mrow[:, nb * 512:(nb + 1) * 512], pb[0:2, :], bm[:, nb * 512:(nb + 1) * 512], ALU.add, [pk, 'bm'], ['mrow'])
            dma('sp', m_scr, mrow[:], reads=['mrow'], writes=['m_scr'])
        S.barrier()

        with ExitStack() as st:
            AT = sbt(st, "AT", [128, 8, NTOK], BF16)
            with ExitStack() as st1:
                fm = sbt(st1, "fm", [128, 5, 8])
                gs = sbt(st1, "gs", [128, 2, 8])
                dma('sp', fm[:, 0, :], n1w_d.rearrange("(k p) -> p k", p=128), writes=['fm'], allow_slow_non_contiguous=True)
                for j, (r, c) in enumerate([(0, 0), (0, 1), (1, 0), (1, 1)]):
                    dma('sp', fm[:, 1 + j, :], m_scr[r, c * D:(c + 1) * D].rearrange("(k p) -> p k", p=128), reads=['m_scr'], writes=['fm'], allow_slow_non_contiguous=True)
                stt('dve', gs[:, 0, :], fm[:, 2, :], 1.0, fm[:, 0, :], ALU.add, ALU.mult, ['fm'], ['gs'])
                stt('dve', gs[:, 1, :], fm[:, 4, :], 1.0, fm[:, 0, :], ALU.add, ALU.mult, ['fm'], ['gs'])
                xt = [sbt(st1, "xt%d" % i, [128, D]) for i in range(2)]
                junk = sbt(st1, "junk1", [128, D]); tmp = sbt(st1, "tmp1", [128, 8, 128])
                ss = sbt(st1, "ss1", [128, 2])
                for i in range(NT):
                    b = i % 2; xk = "xt%d" % b
                    src = ctx_d[tokrows(i), :] if i < 2 else x_d[tokrows(i - 2), :]
                    dma('sp', xt[b][:], src, writes=[xk])
                    sk = "ss1_%d" % b
                    act(junk[:], xt[b][:], AF.Square, [xk], ['junk1', sk], accum_out=ss[:, b:b + 1])
                    rstd_from(ss[:, b:b + 1], D, 1e-6, sk)
                    ts('dve', xt[b][:], xt[b][:], ss[:, b:b + 1], None, ALU.mult, None, [xk, sk], [xk])
                    for k in range(8):
                        pb, pk = bank((i % 2) * 2 + k // 4)
                        tr(pb[:, (k % 4) * 128:(k % 4 + 1) * 128], xt[b][:, k * 128:(k + 1) * 128], [xk], [pk])
                    g = 1 if i < 2 else 0
                    sh = 3 if i < 2 else 1
                    for hlf in range(2):
                        pb, pk = bank((i % 2) * 2 + hlf)
                        pv = pb.rearrange("p (k t) -> p k t", k=4)
                        tt('dve', tmp[:, hlf * 4:(hlf + 1) * 4, :], pv, gs[:, g, hlf * 4:(hlf + 1) * 4].unsqueeze(2).to_broadcast([128, 4, 128]),
                           ALU.mult, [pk, 'gs'], ['tmp1'])
                        tt('dve', AT[:, hlf * 4:(hlf + 1) * 4, tokrows(i)], tmp[:, hlf * 4:(hlf + 1) * 4, :],
                           fm[:, sh, hlf * 4:(hlf + 1) * 4].unsqueeze(2).to_broadcast([128, 4, 128]), ALU.add, ['tmp1', 'fm'], ['AT'])
            S.barrier()
            with ExitStack() as st2:
                wf = [sbt(st2, "wf%d" % i, [128, 8, 512]) for i in range(2)]
                wbf = [sbt(st2, "wbf%d" % i, [128, 8, 512], BF16) for i in range(2)]
                zt = [sbt(st2, "zt%d" % i, [128, 512]) for i in range(4)]
                cnt = 0
                for nb in range(17):
                    n0 = nb * 512; nw = min(512, INC - n0)
                    b = nb % 2
                    dma('sp' if b == 0 else 'act', wf[b][:, :, 0:nw], win_d[:, n0:n0 + nw].rearrange("(k p) n -> p k n", p=128), writes=["wf%d" % b])
                    cp('pool', wbf[b][:, 0:4, 0:nw], wf[b][:, 0:4, 0:nw], ["wf%d" % b], ["wbf%d" % b])
                    cp('dve', wbf[b][:, 4:8, 0:nw], wf[b][:, 4:8, 0:nw], ["wf%d" % b], ["wbfb%d" % b])
                    for i in range(NT):
                        pb, pk = bank(cnt % 8)
                        for k in range(8):
                            mm(pb[:, 0:nw], AT[:, k, tokrows(i)], wbf[b][:, k, 0:nw], ['AT', "wbf%d" % b, "wbfb%d" % b], [pk], start=(k == 0), stop=(k == 7))
                        zb = cnt % 4
                        if cnt % 2 == 0:
                            act(zt[zb][:, 0:nw], pb[:, 0:nw], AF.Identity, [pk], ["zt%d" % zb])
                        else:
                            cp('dve', zt[zb][:, 0:nw], pb[:, 0:nw], [pk], ["zt%d" % zb])
                        dma('sp', z_scr[tokrows(i), n0:n0 + nw], zt[zb][:, 0:nw], reads=["zt%d" % zb], writes=[])
                        cnt += 1
        S.barrier()
        if 'z' in dbg_d:
            for i in range(NT):
                dma('sp', dbg_d['z'][tokrows(i), :], z_scr[tokrows(i), :])
            S.barrier()
        if stop == 'p2':
            return nc

        with ExitStack() as st:
            WA = sbt(st, "WA", [33, 1024]); gnw = sbt(st, "gnw", [128, D])
            dma('sp', WA[:], wa_d, writes=['WA'])
            dma('sp', gnw[:], gnw_d.partition_broadcast(128), writes=['gnw'])
            zqk = sbt(st, "zqk", [128, 1024]); zv = sbt(st, "zv", [128, 1024]); zalo = sbt(st, "zalo", [128, 32])
            aloT = sbt(st, "aloT", [33, 128])
            op('dve', lambda e: e.memset(aloT[:], 1.0), writes=['aloT'])
            e1 = sbt(st, "e1", [128, 512]); sp_ = sbt(st, "sp", [128, 512]); Em = sbt(st, "Em", [128, 512])
            EpT = sbt(st, "EpT", [128, 512]); EmT = sbt(st, "EmT", [128, 512]); el = sbt(st, "el", [128, 4])
            kd = sbt(st, "kd", [128, 512], BF16); qdT = sbt(st, "qdT", [128, 512], BF16); kdT = sbt(st, "kdT", [128, 512], BF16)
            vb = sbt(st, "vb", [128, 1024], BF16); attb = sbt(st, "attb", [128, 512], BF16)
            Sf = [sbt(st, "Sf%d" % d, [128, 1024]) for d in range(2)]
            Sb = [sbt(st, "Sb%d" % d, [128, 1024], BF16) for d in range(2)]
            og = sbt(st, "og", [128, 1024]); ogf = sbt(st, "ogf", [128, 1024]); sq = sbt(st, "sqg", [128, 1024])
            zg = sbt(st, "zg", [128, 1024]); zgg = sbt(st, "zgg", [128, 1024]); rs4 = sbt(st, "rs4", [128, 4])
            for d in range(2):
                op('dve', lambda e: e.memset(Sf[d][:], 0.0), writes=["Sf%d" % d])
                op('pool', lambda e: e.memset(Sb[d][:], 0.0), writes=["Sb%d" % d])
            for d in range(2):
                order = list(range(NT)) if d == 0 else [1, 0] + list(range(NT - 1, 1, -1))
                last = 127 if d == 0 else 0
                Matt = M_le if d == 0 else M_ge
                SfK, SbK = "Sf%d" % d, "Sb%d" % d
                for i in order:
                    lat = i >= 2
                    li = i - 2
                    dma('sp', zqk[:], z_scr[tokrows(i), 0:1024], reads=[], writes=['zqk'])
                    dma('act', zv[:], z_scr[tokrows(i), 1024:2048], reads=[], writes=['zv'])
                    dma('sp', zalo[:], z_scr[tokrows(i), 3072:3104], reads=[], writes=['zalo'])
                    if lat and d == 1:
                        dma('act', zg[:], z_scr[tokrows(i), 2048:3072], reads=[], writes=['zg'])
                        dma('sp', zgg[:], z_scr[tokrows(i), GATE0:GATE0 + 1024], reads=[], writes=['zgg'])
                        dma('act', ogf[:], ogf_scr[tokrows(li), :], reads=['ogf%d' % li], writes=['ogf'])
                    p0, k0 = bank(0)
                    tr(p0[0:32, 0:128], zalo[:, 0:32], ['zalo'], [k0])
                    cp('dve', aloT[0:32, :], p0[0:32, 0:128], [k0], ['aloT'])
                    p1, k1 = bank(1)
                    mm(p1, aloT[:], WA[:, d * 512:(d + 1) * 512], ['aloT', 'WA'], [k1])
                    act(e1[:], p1, AF.Exp, [k1], ['e1'], scale=-1.0)
                    act(sp_[:], e1[:], AF.Ln, ['e1'], ['sp'], bias=1.0)
                    p2, k2 = bank(2)
                    mm(p2, gcum[d], sp_[:], ['sp'], [k2])
                    act(Em[:], p2, AF.Exp, [k2], ['Em'], scale=-1.0)
                    tt('dve', kd[:], zqk[:, 512:1024], Em[:], ALU.mult, ['zqk', 'Em'], ['kd'])
                    p3, k3 = bank(3)
                    for h in range(4):
                        mm(p3[:, h * 128:(h + 1) * 128], sp_[:, h * 128:(h + 1) * 128], gcum[d], ['sp'], [k3])
                    act(EpT[:], p3, AF.Exp, [k3], ['EpT'])
                    act(EmT[:], p3, AF.Exp, [k3], ['EmT'], scale=-1.0)
                    cp('dve', el[:], EpT[:].rearrange("p (h t) -> p h t", h=4)[:, :, last], ['EpT'], ['el'])
                    p4, k4 = bank(4); p5, k5 = bank(5)
                    for h in range(4):
                        tr(p4[:, h * 128:(h + 1) * 128], zqk[:, h * 128:(h + 1) * 128], ['zqk'], [k4])
                        tr(p5[:, h * 128:(h + 1) * 128], zqk[:, 512 + h * 128:512 + (h + 1) * 128], ['zqk'], [k5])
                    stt('dve', qdT[:], p4, float(128 ** -0.5), EpT[:], ALU.mult, ALU.mult, [k4, 'EpT'], ['qdT'])
                    tt('dve', kdT[:], p5, EmT[:], ALU.mult, [k5, 'EmT'], ['kdT'])
                    cp('pool', vb[:], zv[:], ['zv'], ['vb'])
                    if lat:
                        p6, k6 = bank(6)
                        for h in range(4):
                            mm(p6[:, h * 128:(h + 1) * 128], kdT[:, h * 128:(h + 1) * 128], qdT[:, h * 128:(h + 1) * 128], ['kdT', 'qdT'], [k6])
                        tt('dve', attb[:].rearrange("p (h t) -> p h t", h=4), p6.rearrange("p (h t) -> p h t", h=4),
                           Matt.unsqueeze(1).to_broadcast([128, 4, 128]), ALU.mult, [k6, 'cst'], ['attb'])
                        for h in range(4):
                            pb, pk = bank(h // 2)
                            o_ap = pb[:, (h % 2) * 256:(h % 2 + 1) * 256]
                            mm(o_ap, attb[:, h * 128:(h + 1) * 128], vb[:, h * 256:(h + 1) * 256], ['attb', 'vb'], [pk], start=True, stop=False)
                            mm(o_ap, qdT[:, h * 128:(h + 1) * 128], Sb[d][:, h * 256:(h + 1) * 256], ['qdT', SbK], [pk], start=False, stop=True)
                        if d == 0:
                            cp('dve', og[:, 0:512], bank(0)[0], ['ps0'], ['og'])
                            act(og[:, 512:1024], bank(1)[0], AF.Identity, ['ps1'], ['og'])
                            dma('sp', ogf_scr[tokrows(li), :], og[:], reads=['og'], writes=['ogf%d' % li])
                        else:
                            tt('dve', og[:, 0:512], bank(0)[0], ogf[:, 0:512], ALU.add, ['ps0', 'ogf'], ['og'])
                            tt('dve', og[:, 512:1024], bank(1)[0], ogf[:, 512:1024], ALU.add, ['ps1', 'ogf'], ['og'])
                            dump('og_%d' % li, og[:], 'og')
                            tt('pool', sq[:], og[:], og[:], ALU.mult, ['og'], ['sqg'])
                            red(rs4[:], sq[:].rearrange("p (h e) -> p h e", h=4), ['sqg'], ['rs4'])
                            rstd_from(rs4[:], 256, 1e-6, 'rs4')
                            tt('dve', og[:].rearrange("p (h e) -> p h e", h=4), og[:].rearrange("p (h e) -> p h e", h=4),
                               rs4[:].unsqueeze(2).to_broadcast([128, 4, 256]), ALU.mult, ['og', 'rs4'], ['og'])
                            tt('dve', og[:], og[:], gnw[:], ALU.mult, ['og', 'gnw'], ['og'])
                            act(zg[:], zg[:], AF.Silu, ['zg'], ['zg'])
                            act(zgg[:], zgg[:], AF.Sigmoid, ['zgg'], ['zgg'])
                            tt('pool', zg[:], zg[:], zgg[:], ALU.mult, ['zg', 'zgg'], ['zg'])
                            tt('dve', og[:], og[:], zg[:], ALU.mult, ['og', 'zg'], ['og'])
                            dma('sp', ygla_scr[tokrows(li), :], og[:], reads=['og'], writes=['ygla%d' % li])
                    for h in range(4):
                        pb, pk = bank(2 + h // 2)
                        mm(pb[:, (h % 2) * 256:(h % 2 + 1) * 256], kd[:, h * 128:(h + 1) * 128], vb[:, h * 256:(h + 1) * 256], ['kd', 'vb'], [pk])
                    tt('dve', Sf[d][:, 0:512], bank(2)[0], Sf[d][:, 0:512], ALU.add, ['ps2', SfK], [SfK])
                    tt('dve', Sf[d][:, 512:1024], bank(3)[0], Sf[d][:, 512:1024], ALU.add, ['ps3', SfK], [SfK])
                    tt('dve', Sf[d][:].rearrange("p (h e) -> p h e", h=4), Sf[d][:].rearrange("p (h e) -> p h e", h=4),
                       el[:].unsqueeze(2).to_broadcast([128, 4, 256]), ALU.mult, [SfK, 'el'], [SfK])
                    act(Sb[d][:], Sf[d][:], AF.Identity, [SfK], [SbK])
                    if i == 0 and d == 1:
                        dump('sg_b', Sf[1][:], SfK)
                    if i == 1 and d == 0:
                        dump('sg_f', Sf[0][:], SfK)
        S.barrier()
        if 'ygla' in dbg_d:
            for i in range(16):
                dma('sp', dbg_d['ygla'][tokrows(i), :], ygla_scr[tokrows(i), :])
            S.barrier()
        if stop == 'p3':
            return nc

        with ExitStack() as st:
            W2A = [sbt(st, "W2A%d" % d, [65, D]) for d in range(2)]
            A2A = sbt(st, "A2A", [65, D]); G2a = sbt(st, "G2a", [128, D]); G2b = sbt(st, "G2b", [32, D])
            for d in range(2):
                dma('sp', W2A[d][:], w2a_d[d], writes=["W2A%d" % d])
            dma('sp', A2A[:], a2a_d, writes=['A2A'])
            dma('sp', G2a[:], g2_d[0:128, :], writes=['G2a']); dma('sp', G2b[:], g2_d[128:160, :], writes=['G2b'])
            bcs = {}
            for nm, src in [('kk', kk_d), ('ka', ka_d), ('rk', rk_d), ('lnw', lnw_d), ('lnb', lnb_d)]:
                bcs[nm] = sbt(st, "bc_" + nm, [128, D])
                dma('act', bcs[nm][:], src.partition_broadcast(128), writes=['bc_' + nm])
            zm = sbt(st, "zm", [128, RWC]); zc = sbt(st, "zc", [128, 856]); muc = sbt(st, "muc", [128, 856])
            shb = [sbt(st, "shb%d" % j, [128, 856]) for j in range(2)]
            for j in range(2):
                op('pool', lambda e: e.memset(shb[j][:], 0.0), writes=["shb%d" % j])
            G = [sbt(st, "g%d" % j, [128, D]) for j in range(13)]
            gk = ["g%d" % j for j in range(13)]
            XT = [sbt(st, "XT%d" % j, [128, 1024], F32R if FAST32 else F32) for j in range(4)]
            for j in range(4):
                op('dve', lambda e: e.memset(XT[j][:].bitcast(F32), 0.0), writes=["XT%d_0" % j, "XT%d_1" % j])
            XYW = [sbt(st, "xyw%d" % j, [128, 1024], F32R if FAST32 else F32) for j in range(6)]
            xk = ["xyw%d" % j for j in range(6)]
            G0 = [sbt(st, "G0_%d" % d, [128, D]) for d in range(2)]
            for d in range(2):
                op('dve', lambda e: e.memset(G0[d][:], 0.0), writes=["G0_%d" % d])
            gam = sbt(st, "gam", [64, 256]); negc = sbt(st, "negc", [128, 16]); tw = sbt(st, "tw", [128, 64]); sg = sbt(st, "sgl", [128, 160])
            lt = sbt(st, "lt", [65, 128]); la = sbt(st, "la", [65, 128])
            op('dve', lambda e: e.memset(negc[:], -C0), writes=['negc'])
            op('dve', lambda e: e.memset(lt[:], 1.0), writes=['lt']); op('dve', lambda e: e.memset(la[:], 1.0), writes=['la'])
            s16 = sbt(st, "s16", [128, 16]); gT0 = sbt(st, "gT0", [128, 128]); gT1 = sbt(st, "gT1", [32, 128])

            cvf = [sbt(st, "cvf%d" % j, [128, 2, D]) for j in range(2)]
            cvb = [sbt(st, "cvb%d" % j, [128, 2, D], BF16) for j in range(2)]
            cv_state = [0]

            def convert_chunks(n):
                for _ in range(n):
                    c = cv_state[0]
                    if c >= 128:
                        return
                    cv_state[0] += 1
                    src, c_off = (pu_d, 0) if c < 64 else (pv_d, D)
                    r0 = (c % 64) * 256
                    dst = uv16_scr[:, c_off:c_off + D]
                    b = c % 2
                    dma('sp', cvf[b][:], src[r0:r0 + 256, :].rearrange("(p r) d -> p r d", r=2), writes=["cvf%d" % b])
                    cp('pool', cvb[b][:], cvf[b][:], ["cvf%d" % b], ["cvb%d" % b])
                    dma('sp', dst[r0:r0 + 256, :].rearrange("(p r) d -> p r d", r=2), cvb[b][:], reads=["cvb%d" % b])

            def preg(b0, nb):
                t = psA if b0 < 4 else psB
                j = b0 % 4
                return t[:, j * 512:(j + nb) * 512], pkeys(b0, nb)

            def v3(ap, h):
                return ap.rearrange("p (h t) -> p h t", h=h)

            for d in range(2):
                order = list(range(NT)) if d == 0 else [1, 0] + list(range(NT - 1, 1, -1))
                Mst = M_lt if d == 0 else M_gt
                MstT = M_gt if d == 0 else M_lt
                Min = M_le if d == 0 else M_ge
                G0k = "G0_%d" % d
                for i in order:
                    lat = i >= 2
                    li = i - 2
                    R0 = i * 128
                    if lat:
                        specs = [(-1, 0), (1, 1), (-64, 2 if li == 0 else 4), (64, 3 if li == 15 else 4)]
                    else:
                        specs = [(-1, 5 if i == 0 else 4), (1, 6 if i == 1 else 4)] * 2
                    if d == 0:
                        for c in range(4):
                            c0 = RW0 + c * 856
                            dma('sp', zc[:], z_scr[R0:R0 + 128, c0:c0 + 856], writes=['zc'])
                            dma('act', muc[:], mu_d[c * 856:(c + 1) * 856].partition_broadcast(128), writes=['muc'])
                            zmc = zm[:, c * 856:(c + 1) * 856]
                            for j in range(4):
                                off, mcol = specs[j]
                                lo = R0 + off
                                clo, chi = max(lo, 0), min(lo + 128, NTOK)
                                sb_ = shb[j % 2]; sbk = "shb%d" % (j % 2)
                                dma('sp' if j % 2 == 0 else 'act', sb_[clo - lo:chi - lo, :], z_scr[clo:chi, c0:c0 + 856], writes=[sbk])
                                stt('dve', zmc.rearrange("p (f j) -> p f j", j=4)[:, :, j], sb_[:].rearrange("p (f j) -> p f j", j=4)[:, :, j],
                                    shm[:, mcol:mcol + 1], zc[:].rearrange("p (f j) -> p f j", j=4)[:, :, j], ALU.mult, ALU.subtract,
                                    [sbk, 'zc', 'cst'], ['zm'])
                            tt('dve', zmc, zmc, muc[:], ALU.mult, ['zm', 'muc'], ['zm'])
                            tt('dve', zmc, zmc, zc[:], ALU.add, ['zm', 'zc'], ['zm'])
                        dma('sp', zm_scr[R0:R0 + 128, :], zm[:], reads=['zm'], writes=['zmscr%d' % i])
                    else:
                        dma('sp', zm[:], zm_scr[R0:R0 + 128, :], reads=['zmscr%d' % i], writes=['zm'])
                    if i == 2 and d == 0:
                        dump('zm_2', zm[:], 'zm')
                    r_ = zm[:, 0:1024]; kr = zm[:, 1024:2048]; vr = zm[:, 2048:3072]
                    convert_chunks(4)

                    if rstop == 1:
                        S.barrier()
                        return nc
                    sig, a_, kk_, km_, b_, Ecum, Eneg, Epv, Rt, Kt, Bt, Pt, tmpb = G
                    act(tw[:], zm[:, 3072 + d * 64:3072 + (d + 1) * 64], AF.Tanh, ['zm'], ['tw'])
                    p0, k0 = bank(0)
                    tr(p0[0:64, 0:128], tw[:], ['tw'], [k0])
                    cp('dve', lt[0:64, :], p0[0:64, 0:128], [k0], ['lt'])
                    tr(p0[0:64, 128:256], zm[:, 3200:3264], ['zm'], [k0])
                    cp('dve', la[0:64, :], p0[0:64, 128:256], [k0], ['la'])
                    for hf in range(2):
                        pb, pk = bank(1 + hf)
                        mm(pb, lt[:], W2A[d][:, hf * 512:(hf + 1) * 512], ['lt', "W2A%d" % d], [pk])
                        act(sig[:, hf * 512:(hf + 1) * 512], pb, AF.Sigmoid, [pk], [gk[0]])
                        pb2, pk2 = bank(3 + hf)
                        mm(pb2, la[:], A2A[:, hf * 512:(hf + 1) * 512], ['la', 'A2A'], [pk2])
                        act(a_[:, hf * 512:(hf + 1) * 512], pb2, AF.Sigmoid, [pk2], [gk[1]])
                    for hf in range(2):
                        pb, pk = bank(5 + hf)
                        hs = slice(hf * 512, (hf + 1) * 512)
                        mm(pb, rcum[d], sig[:, hs], [gk[0]], [pk])
                        act(Ecum[:, hs], pb, AF.Exp, [pk], [gk[5]])
                        act(Eneg[:, hs], pb, AF.Exp, [pk], [gk[6]], scale=-1.0)
                        stt('dve', Epv[:, hs], sig[:, hs], C0, pb, ALU.mult, ALU.add, [gk[0], pk], [gk[7]])
                    act(Epv[:], Epv[:], AF.Exp, [gk[7]], [gk[7]])
                    p7, k7 = bank(7)
                    for h in range(16):
                        mm(p7[0:64, 16 * h:16 * h + 16], sig[:, h * 64:(h + 1) * 64], negc[:], [gk[0], 'negc'], [k7])
                    act(gam[:], p7[0:64, 0:256], AF.Exp, [k7], ['gam'])

                    if rstop == 2:
                        S.barrier()
                        return nc
                    tt('dve', kk_[:], kr, bcs['kk'][:], ALU.mult, ['zm', 'bc_kk'], [gk[2]])
                    tt('dve', tmpb[:], kk_[:], kk_[:], ALU.mult, [gk[2]], [gk[12]])
                    red(s16[:], v3(tmpb[:], 16), [gk[12]], ['s16'])
                    act(s16[:], s16[:], AF.Sqrt, ['s16'], ['s16'])
                    ts('dve', s16[:], s16[:], 1e-12, None, ALU.max, None, ['s16'], ['s16'])
                    op('dve', lambda e: e.reciprocal(out=s16[:], in_=s16[:]), reads=['s16'], writes=['s16'])
                    tt('dve', v3(kk_[:], 16), v3(kk_[:], 16), s16[:].unsqueeze(2).to_broadcast([128, 16, 64]), ALU.mult, [gk[2], 's16'], [gk[2]])
                    stt('dve', km_[:], a_[:], -1.0, bcs['ka'][:], ALU.add, ALU.mult, [gk[1], 'bc_ka'], [gk[3]])
                    stt('dve', km_[:], km_[:], 1.0, kr, ALU.add, ALU.mult, [gk[3], 'zm'], [gk[3]])
                    tt('dve', b_[:], a_[:], kk_[:], ALU.mult, [gk[1], gk[2]], [gk[4]])
                    tt('dve', Rt[:], r_, Ecum[:], ALU.mult, ['zm', gk[5]], [gk[8]])
                    tt('dve', Kt[:], km_[:], Eneg[:], ALU.mult, [gk[3], gk[6]], [gk[9]])
                    tt('dve', Bt[:], b_[:], Eneg[:], ALU.mult, [gk[4], gk[6]], [gk[10]])
                    tt('dve', Pt[:], kk_[:], Epv[:], ALU.mult, [gk[2], gk[7]], [gk[11]])

                    if rstop == 3:
                        S.barrier()
                        return nc
                    AK, RK, RB, Xin, Uneg, o_ = G[0], G[1], G[2], G[4], G[5], G[6]
                    AKk, RKk, RBk, Xink, Unk, ok = gk[0], gk[1], gk[2], gk[4], gk[5], gk[6]
                    for hh in range(2):
                        for q, (src, sk_) in enumerate([(Rt, gk[8]), (Kt, gk[9]), (Bt, gk[10]), (Pt, gk[11])]):
                            reg, rks = preg(2 * q, 2)
                            for j in range(8):
                                h = hh * 8 + j
                                tr(reg[0:64, j * 128:(j + 1) * 128], src[:, h * 64:(h + 1) * 64], [sk_], [rks[j // 4]])
                            for bnk in range(2):
                                bs = slice(bnk * 512, (bnk + 1) * 512)
                                if q % 2 == 0:
                                    act(XT[q][0:64, bs], reg[0:64, bs], AF.Identity, [rks[bnk]], ["XT%d_%d" % (q, bnk)])
                                else:
                                    cp('dve', XT[q][0:64, bs], reg[0:64, bs], [rks[bnk]], ["XT%d_%d" % (q, bnk)])

                        if rstop == 4:
                            S.barrier()
                            return nc
                        RT, KT, BT, PT = XT
                        Xa, Xb, Ya, Yb, Wa, Wb = XYW

                        def hs128(ap, j):
                            return ap[:, j * 128:(j + 1) * 128]
                        plan = [(0, BT, PT, 'XT2', 'XT3', Xa, xk[0], Mst), (2, PT, BT, 'XT3', 'XT2', Ya, xk[2], MstT),
                                (4, KT, PT, 'XT1', 'XT3', AK, AKk, Mst)]
                        if lat:
                            plan += [(6, KT, RT, 'XT1', 'XT0', RK, RKk, Min), (0, BT, RT, 'XT2', 'XT0', RB, RBk, Min)]
                        for (b0, L, Rr, Lk, Rk_, dst, dk, msk) in plan:
                            reg, rks = preg(b0, 2)
                            for j in range(8):
                                mm(hs128(reg, j), hs128(L, j), hs128(Rr, j), [Lk + "_%d" % (j // 4), Rk_ + "_%d" % (j // 4)], [rks[j // 4]], r32=True)
                            for bnk in range(2):
                                bs = slice(bnk * 512, (bnk + 1) * 512)
                                tt('dve', v3(dst[:, bs], 4), v3(reg[:, bs], 4), msk.unsqueeze(1).to_broadcast([128, 4, 128]), ALU.mult,
                                   [rks[bnk], 'cst'], [dk + "_%d" % bnk if dk.startswith('xyw') else dk])

                        if rstop == 5:
                            S.barrier()
                            return nc
                        stt('dve', v3(Wa[:], 8), v3(Xa[:], 8), -1.0, ident.unsqueeze(1).to_broadcast([128, 8, 128]), ALU.mult, ALU.add,
                            [xk[0] + "_0", xk[0] + "_1", 'cst'], [xk[4] + "_0", xk[4] + "_1"])
                        Xc, Yc, Xn, Yn, Wc, Wn = Xa, Ya, Xb, Yb, Wa, Wb
                        Xck, Yck, Xnk, Ynk, Wck, Wnk = xk[0], xk[2], xk[1], xk[3], xk[4], xk[5]
                        for lev in range(6):
                            regP, kP = preg(2, 2); regQ, kQ = preg(4, 2); regR, kR = preg(6, 2)
                            for half in range(2):
                                hk_ = "_%d" % half
                                if lev < 5:
                                    for j in range(4 * half, 4 * half + 4):
                                        mm(hs128(regP, j), hs128(Yc, j), hs128(Xc, j), [Xck + hk_, Yck + hk_], [kP[half]], r32=True)
                                for j in range(4 * half, 4 * half + 4):
                                    mm(hs128(regQ, j), hs128(Xc, j), hs128(Yc, j), [Xck + hk_, Yck + hk_], [kQ[half]], r32=True)
                            for bnk in range(2):
                                bs = slice(bnk * 512, (bnk + 1) * 512)
                                hk_ = "_%d" % bnk
                                if lev < 5:
                                    act(Xn[:, bs], regP[:, bs], AF.Identity, [kP[bnk]], [Xnk + hk_])
                                cp('dve', Yn[:, bs], regQ[:, bs], [kQ[bnk]], [Ynk + hk_])
                            for j in range(8):
                                hk_ = "_%d" % (j // 4)
                                mm(hs128(regR, j), hs128(Yn, j), hs128(Wc, j), [Ynk + hk_, Wck + hk_], [kR[j // 4]], r32=True)
                            for bnk in range(2):
                                bs = slice(bnk * 512, (bnk + 1) * 512)
                                hk_ = "_%d" % bnk
                                tt('dve', Wn[:, bs], regR[:, bs], Wc[:, bs], ALU.add, [kR[bnk], Wck + hk_], [Wnk + hk_])
                            Xc, Xn, Xck, Xnk = Xn, Xc, Xnk, Xck
                            Yc, Yn, Yck, Ynk = Yn, Yc, Ynk, Yck
                            Wc, Wn, Wck, Wnk = Wn, Wc, Wnk, Wck

                        if rstop == 6:
                            S.barrier()
                            return nc
                        hsl = slice(hh * 512, (hh + 1) * 512)
                        p0, k0 = bank(0); p1, k1 = bank(1)
                        for j in range(8):
                            h = hh * 8 + j
                            o64 = p0[:, j * 64:(j + 1) * 64]
                            mm(o64, hs128(PT, j), G0[d][:, h * 64:(h + 1) * 64], ['XT3_%d' % (j // 4), G0k], [k0], start=True, stop=False)
                            mm(o64, hs128(AK, j), vr[:, h * 64:(h + 1) * 64], [AKk, 'zm'], [k0], start=False, stop=True)
                        cp('dve', Xin[:, hsl], p0, [k0], [Xink])
                        if rstop == 61:
                            S.barrier()
                            return nc
                        for j in range(8):
                            h = hh * 8 + j
                            mm(p1[:, j * 64:(j + 1) * 64], hs128(Wc, j), Xin[:, h * 64:(h + 1) * 64], [Wck + "_%d" % (j // 4), Xink], [k1])
                        if rstop == 620:
                            S.barrier()
                            return nc
                        ts('dve', Uneg[:, hsl], p1, -1.0, None, ALU.mult, None, [k1], [Unk])
                        if rstop == 62:
                            S.barrier()
                            return nc
                        if lat:
                            for j in range(8):
                                h = hh * 8 + j
                                o64 = p0[:, j * 64:(j + 1) * 64]
                                mm(o64, hs128(RT, j), G0[d][:, h * 64:(h + 1) * 64], ['XT0_%d' % (j // 4), G0k], [k0], start=True, stop=False)
                                mm(o64, hs128(RK, j), vr[:, h * 64:(h + 1) * 64], [RKk, 'zm'], [k0], start=False, stop=False)
                                mm(o64, hs128(RB, j), Uneg[:, h * 64:(h + 1) * 64], [RBk, Unk], [k0], start=False, stop=True)
                            act(o_[:, hsl], p0, AF.Identity, [k0], [ok])
                        for j in range(8):
                            h = hh * 8 + j
                            o64 = p1[0:64, j * 64:(j + 1) * 64]
                            mm(o64, Kt[:, h * 64:(h + 1) * 64], vr[:, h * 64:(h + 1) * 64], [gk[9], 'zm'], [k1], start=True, stop=False)
                            mm(o64, Bt[:, h * 64:(h + 1) * 64], Uneg[:, h * 64:(h + 1) * 64], [gk[10], Unk], [k1], start=False, stop=True)
                        tt('dve', G0[d][0:64, hsl], p1[0:64, :], G0[d][0:64, hsl], ALU.add, [k1, G0k], [G0k])
                        tt('dve', v3(G0[d][0:64, hsl], 8), v3(G0[d][0:64, hsl], 8),
                           gam[:].rearrange("p (h x) -> p h x", x=16)[:, hh * 8:(hh + 1) * 8, 0:1].to_broadcast([64, 8, 64]),
                           ALU.mult, [G0k, 'gam'], [G0k])
                        if rstop == 63:
                            S.barrier()
                            return nc
                    if rstop == 7:
                        S.barrier()
                        return nc
                    if i == 1 and d == 0:
                        dump('sr_f', G0[0][0:64, :], G0k)
                    if rstop == 8 and i == 1 and d == 0:
                        S.barrier()
                        return nc
                    if not lat:
                        continue
                    if d == 0:
                        if li == 0:
                            dump('orf_0', o_[:], ok)
                        dma('sp', orf_scr[tokrows(li), :], o_[:], reads=[ok], writes=['orf%d' % li])
                        if rstop == 9:
                            S.barrier()
                            return nc
                        if rstop == 10 and li == 15:
                            S.barrier()
                            return nc
                        continue
                    orf, ygl, zgr, tmp2 = G[7], G[8], G[9], G[10]
                    dma('sp', orf[:], orf_scr[tokrows(li), :], reads=['orf%d' % li], writes=[gk[7]])
                    dma('act', ygl[:], ygla_scr[tokrows(li), :], writes=[gk[8]])
                    dma('sp', zgr[:], z_scr[tokrows(i), GATE0 + 1024:GATE0 + 2048], writes=[gk[9]])
                    tt('dve', o_[:], o_[:], orf[:], ALU.add, [ok, gk[7]], [ok])
                    if li == 0:
                        dump('orsum_0', o_[:], ok)
                    red(s16[:], v3(o_[:], 16), [ok], ['s16'])
                    ts('dve', s16[:], s16[:], 1.0 / 64, None, ALU.mult, None, ['s16'], ['s16'])
                    tt('dve', v3(o_[:], 16), v3(o_[:], 16), s16[:].unsqueeze(2).to_broadcast([128, 16, 64]), ALU.subtract, [ok, 's16'], [ok])
                    tt('dve', tmp2[:], o_[:], o_[:], ALU.mult, [ok], [gk[10]])
                    red(s16[:], v3(tmp2[:], 16), [gk[10]], ['s16'])
                    rstd_from(s16[:], 64, 64e-5, 's16')
                    tt('dve', v3(o_[:], 16), v3(o_[:], 16), s16[:].unsqueeze(2).to_broadcast([128, 16, 64]), ALU.mult, [ok, 's16'], [ok])
                    tt('dve', o_[:], o_[:], bcs['lnw'][:], ALU.mult, [ok, 'bc_lnw'], [ok])
                    tt('dve', o_[:], o_[:], bcs['lnb'][:], ALU.add, [ok, 'bc_lnb'], [ok])
                    tt('dve', tmp2[:], r_, km_[:], ALU.mult, ['zm', gk[3]], [gk[10]])
                    tt('dve', tmp2[:], tmp2[:], bcs['rk'][:], ALU.mult, [gk[10], 'bc_rk'], [gk[10]])
                    red(s16[:], v3(tmp2[:], 16), [gk[10]], ['s16'])
                    tt('dve', v3(tmp2[:], 16), v3(vr, 16), s16[:].unsqueeze(2).to_broadcast([128, 16, 64]), ALU.mult, ['zm', 's16'], [gk[10]])
                    tt('dve', o_[:], o_[:], tmp2[:], ALU.add, [ok, gk[10]], [ok])
                    act(sg[:], zm[:, 3264:3424], AF.Sigmoid, ['zm'], ['sgl'])
                    p2, k2 = bank(2)
                    tr(p2[:, 0:128], sg[:, 0:128], ['sgl'], [k2])
                    tr(p2[0:32, 128:256], sg[:, 128:160], ['sgl'], [k2])
                    cp('dve', gT0[:], p2[:, 0:128], [k2], ['gT0'])
                    cp('dve', gT1[:], p2[0:32, 128:256], [k2], ['gT1'])
                    for hf in range(2):
                        pb, pk = bank(3 + hf)
                        hs = slice(hf * 512, (hf + 1) * 512)
                        mm(pb, gT0[:], G2a[:, hs], ['gT0', 'G2a'], [pk], start=True, stop=False)
                        mm(pb, gT1[:], G2b[:, hs], ['gT1', 'G2b'], [pk], start=False, stop=True)
                        tt('dve', o_[:, hs], o_[:, hs], pb, ALU.mult, [ok, pk], [ok])
                    act(zgr[:], zgr[:], AF.Sigmoid, [gk[9]], [gk[9]])
                    tt('dve', o_[:], o_[:], zgr[:], ALU.mult, [ok, gk[9]], [ok])
                    tt('dve', o_[:], o_[:], ygl[:], ALU.add, [ok, gk[8]], [ok])
                    dma('sp', ymix_scr[tokrows(li), :], o_[:], reads=[ok], writes=['ymix%d' % li])
                    if rstop == 11:
                        S.barrier()
                        return nc
            convert_chunks(128)
        S.barrier()
        if 'ymix' in dbg_d:
            for i in range(16):
                dma('sp', dbg_d['ymix'][tokrows(i), :], ymix_scr[tokrows(i), :])
            S.barrier()
        if stop == 'p4':
            return nc

        with ExitStack() as st:
            Wo = sbt(st, "Wo", [128, 8, 1024], BF16); Wq = sbt(st, "Wq", [128, 8, 2048], BF16)
            SKT = sbt(st, "SKT", [128, 16, 128], BF16)
            with ExitStack() as st0:
                stg = [sbt(st0, "stg%d" % j, [128, 8, 512]) for j in range(2)]
                n = 0
                for (src, dst, nblk) in [(wout_d, Wo, 2), (wq_d, Wq, 4)]:
                    for blk in range(nblk):
                        sgb = stg[n % 2]; sgk = "stg%d" % (n % 2)
                        dma('sp' if n % 2 == 0 else 'act', sgb[:], src[:, blk * 512:(blk + 1) * 512].rearrange("(k p) n -> p k n", p=128), writes=[sgk])
                        cp('dve', dst[:, :, blk * 512:(blk + 1) * 512], sgb[:], [sgk], ['W' + str(n)])
                        n += 1
                skf = sbt(st0, "skf", [128, 16, 128])
                dma('sp', skf[:], sk_d.rearrange("b n d -> n b d"), writes=['skf'])
                for blk in range(16):
                    pb, pk = bank(blk // 4)
                    tr(pb[:, (blk % 4) * 128:(blk % 4 + 1) * 128], skf[:, blk, :], ['skf'], [pk])
                for bnk in range(4):
                    cp('dve', SKT[:, bnk * 4:(bnk + 1) * 4, :].rearrange("p b n -> p (b n)"), bank(bnk)[0], [bank(bnk)[1]], ['SKT'])
                S.barrier()
            bc2 = {}
            for nm, src in [('m2', m_scr[0, 2 * D:3 * D]), ('sh2', m_scr[0, 3 * D:4 * D]), ('g2', m_scr[0, 4 * D:5 * D]),
                            ('m5', m_scr[0, 5 * D:6 * D]), ('fnw', fnw_d)]:
                bc2[nm] = sbt(st, "b2_" + nm, [128, D])
                dma('sp', bc2[nm][:], src.partition_broadcast(128), writes=['b2_' + nm])
            with ExitStack() as stn:
                n2wt = sbt(stn, "b2_n2w", [128, D])
                dma('sp', n2wt[:], n2w_d.partition_broadcast(128), writes=['b2_n2w'])
                stt('dve', bc2['g2'][:], bc2['g2'][:], 1.0, n2wt[:], ALU.add, ALU.mult, ['b2_g2', 'b2_n2w'], ['b2_g2'])
                S.barrier()
            ym = sbt(st, "ym", [128, D]); yT = sbt(st, "yT", [128, 8, 128], BF16)
            h1s = [sbt(st, "h1_%d" % j, [128, D]) for j in range(2)]; a2s = [sbt(st, "a2_%d" % j, [128, D]) for j in range(2)]; a2T = sbt(st, "a2T", [128, 8, 128], BF16)
            qT = sbt(st, "qT", [128, 16, 128], BF16); s_sb = sbt(st, "s_sb", [128, 2048]); s2 = sbt(st, "s2", [128, 2048])
            vals = sbt(st, "vals", [128, 16, 16]); idxs = sbt(st, "idxs", [128, 16, 16], U32); idxf = sbt(st, "idxf", [128, 16, 16])
            cand3 = s_sb[:].rearrange("p (h c) -> p h c", h=8); cand23 = s2[:].rearrange("p (h c) -> p h c", h=8)
            tops = sbt(st, "tops", [128, 8, 16]); pos = sbt(st, "pos", [128, 8, 16], U32); posf = sbt(st, "posf", [128, 8, 16])
            pj = sbt(st, "pj", [128, 8, 16]); pi_ = sbt(st, "pi", [128, 8, 16]); sel1 = sbt(st, "sel1", [128, 8, 16]); sel2 = sbt(st, "sel2", [128, 8, 16])
            idx_is = [sbt(st, "idx_i%d" % j, [128, 128], I32) for j in range(2)]; gatess = [sbt(st, "gates%d" % j, [128, 8, 16]) for j in range(2)]; g8 = sbt(st, "gsum8", [128, 8])
            zsc = sbt(st, "zsc", [128, 128]); Aw = sbt(st, "Aw", [128, 128])
            GS = 4
            NGB = 14
            UV = [sbt(st, "UV%d" % j, [128, 2 * D], BF16) for j in range(NGB)]
            prods = [sbt(st, "prod%d" % j, [128, D]) for j in range(2)]
            junk = sbt(st, "junk6", [128, D], BF16); acc = sbt(st, "acc", [128, D])
            identb = sbt(st, "identb", [128, 128], BF16)
            cp('dve', identb[:], ident, ['cst'], ['identb'])
            dg = [sbt(st, "dg%d" % j, [128, GS, 128], BF16) for j in range(2)]
            ss6 = sbt(st, "ss6", [128, 2])
            thr16 = sbt(st, "thr16", [128, 16])
            ts('dve', thr16[:], iota16, 16.0, 16.0, ALU.mult, ALU.add, ['cst'], ['thr16'])
            def front(li):
                fb = li % 2
                dma('sp', ym[:], ymix_scr[tokrows(li), :], writes=['ym'])
                dma('act', h1s[fb][:], x_d[tokrows(li), :], writes=['h1_%d' % fb])
                for k in range(8):
                    pb, pk = bank(k // 4)
                    tr(pb[:, (k % 4) * 128:(k % 4 + 1) * 128], ym[:, k * 128:(k + 1) * 128], ['ym'], [pk])
                for bnk in range(2):
                    cp('dve', yT[:, bnk * 4:(bnk + 1) * 4, :].rearrange("p k t -> p (k t)"), bank(bnk)[0], [bank(bnk)[1]], ['yT'])
                for hf in range(2):
                    pb, pk = bank(2 + hf)
                    hs = slice(hf * 512, (hf + 1) * 512)
                    for k in range(8):
                        mm(pb, yT[:, k, :], Wo[:, k, hs], ['yT', 'W0', 'W1'], [pk], start=(k == 0), stop=(k == 7))
                    tt('dve', a2s[fb][:, hs], pb, bc2['m2'][:, hs], ALU.mult, [pk, 'b2_m2'], ['a2_%d' % fb])
                tt('dve', h1s[fb][:], h1s[fb][:], a2s[fb][:], ALU.add, ['h1_%d' % fb, 'a2_%d' % fb], ['h1_%d' % fb])
                yield
                if li == 0:
                    dump('h1_0', h1s[fb][:], 'h1_%d' % fb)
                act(junk[:], h1s[fb][:], AF.Square, ['h1_%d' % fb], ['junk6', 'ss6a'], accum_out=ss6[:, 0:1])
                rstd_from(ss6[:, 0:1], D, 1e-6, 'ss6a')
                ts('dve', a2s[fb][:], h1s[fb][:], ss6[:, 0:1], None, ALU.mult, None, ['h1_%d' % fb, 'ss6a'], ['a2_%d' % fb])
                tt('dve', a2s[fb][:], a2s[fb][:], bc2['g2'][:], ALU.mult, ['a2_%d' % fb, 'b2_g2'], ['a2_%d' % fb])
                tt('dve', a2s[fb][:], a2s[fb][:], bc2['sh2'][:], ALU.add, ['a2_%d' % fb, 'b2_sh2'], ['a2_%d' % fb])
                for k in range(8):
                    pb, pk = bank(k // 4)
                    tr(pb[:, (k % 4) * 128:(k % 4 + 1) * 128], a2s[fb][:, k * 128:(k + 1) * 128], ['a2_%d' % fb], [pk])
                for bnk in range(2):
                    cp('dve', a2T[:, bnk * 4:(bnk + 1) * 4, :].rearrange("p k t -> p (k t)"), bank(bnk)[0], [bank(bnk)[1]], ['a2T'])
                for blk in range(16):
                    pb, pk = bank(2 + blk // 4)
                    for k in range(8):
                        mm(pb[:, (blk % 4) * 128:(blk % 4 + 1) * 128], Wq[:, k, blk * 128:(blk + 1) * 128], a2T[:, k, :],
                           ['a2T', 'W2', 'W3', 'W4', 'W5'], [pk], start=(k == 0), stop=(k == 7))
                for bnk in range(4):
                    act(qT[:, bnk * 4:(bnk + 1) * 4, :].rearrange("p b t -> p (b t)"), bank(2 + bnk)[0], AF.Identity, [bank(2 + bnk)[1]], ['qT'])
                for blk in range(16):
                    pb, pk = bank(blk // 4)
                    mm(pb[:, (blk % 4) * 128:(blk % 4 + 1) * 128], qT[:, blk, :], SKT[:, blk, :], ['qT', 'SKT'], [pk])
                for bnk in range(4):
                    cp('dve', s_sb[:, bnk * 512:(bnk + 1) * 512], bank(bnk)[0], [bank(bnk)[1]], ['s_sb'])
                for blk in range(16):
                    sv = s_sb[:, blk * 128:(blk + 1) * 128]; s2v = s2[:, blk * 128:(blk + 1) * 128]
                    op('dve', lambda e: e.max(out=vals[:, blk, 0:8], in_=sv), reads=['s_sb'], writes=['vals'])
                    op('dve', lambda e: e.max_index(out=idxs[:, blk, 0:8], in_max=vals[:, blk, 0:8], in_values=sv), reads=['s_sb', 'vals'], writes=['idxs'])
                    op('dve', lambda e: e.match_replace(out=s2v, in_to_replace=vals[:, blk, 0:8], in_values=sv, imm_value=-1e30), reads=['s_sb', 'vals'], writes=['s2'])
                    op('dve', lambda e: e.max(out=vals[:, blk, 8:16], in_=s2v), reads=['s2'], writes=['vals'])
                    op('dve', lambda e: e.max_index(out=idxs[:, blk, 8:16], in_max=vals[:, blk, 8:16], in_values=s2v), reads=['s2', 'vals'], writes=['idxs'])
                    yield
                v4 = vals[:].rearrange("p (h two) s -> p h two s", two=2)
                tt('dve', cand3.rearrange("p h (i j) -> p h i j", i=16), v4[:, :, 0, :].unsqueeze(3).to_broadcast([128, 8, 16, 16]),
                   v4[:, :, 1, :].unsqueeze(2).to_broadcast([128, 8, 16, 16]), ALU.add, ['vals'], ['s_sb'])
                for h in range(8):
                    cv = cand3[:, h, :]; c2v = cand23[:, h, :]
                    op('dve', lambda e: e.max(out=tops[:, h, 0:8], in_=cv), reads=['s_sb'], writes=['tops'])
                    op('dve', lambda e: e.max_index(out=pos[:, h, 0:8], in_max=tops[:, h, 0:8], in_values=cv), reads=['s_sb', 'tops'], writes=['pos'])
                    op('dve', lambda e: e.match_replace(out=c2v, in_to_replace=tops[:, h, 0:8], in_values=cv, imm_value=-1e30), reads=['s_sb', 'tops'], writes=['s2'])
                    op('dve', lambda e: e.max(out=tops[:, h, 8:16], in_=c2v), reads=['s2'], writes=['tops'])
                    op('dve', lambda e: e.max_index(out=pos[:, h, 8:16], in_max=tops[:, h, 8:16], in_values=c2v), reads=['s2', 'tops'], writes=['pos'])
                    yield
                cp('dve', posf[:], pos[:], ['pos'], ['posf'])
                yield
                cp('dve', idxf[:], idxs[:], ['idxs'], ['idxf'])
                tt('dve', s2[:].rearrange("p (h a b) -> p h a b", h=8, a=16), posf[:].unsqueeze(3).to_broadcast([128, 8, 16, 16]),
                   thr16[:].unsqueeze(1).unsqueeze(1).to_broadcast([128, 8, 16, 16]), ALU.is_ge, ['posf', 'thr16'], ['s2'])
                red(pi_[:].rearrange("p h s -> p (h s)"), s2[:].rearrange("p (x b) -> p x b", b=16), ['s2'], ['pi'])
                stt('dve', pj[:], pi_[:], -16.0, posf[:], ALU.mult, ALU.add, ['pi', 'posf'], ['pj'])
                i4 = idxf[:].rearrange("p (h two) s -> p h two s", two=2)
                oh = s2[:].rearrange("p (h a b) -> p h a b", h=8, a=16)
                pr4 = s_sb[:].rearrange("p (h a b) -> p h a b", h=8, a=16)
                for (pp, which, selt, selk) in [(pi_, 0, sel1, 'sel1'), (pj, 1, sel2, 'sel2')]:
                    tt('dve', oh, pp[:].unsqueeze(3).to_broadcast([128, 8, 16, 16]),
                       iota16.unsqueeze(1).unsqueeze(1).to_broadcast([128, 8, 16, 16]), ALU.is_equal, ['pi', 'pj', 'cst'], ['s2'])
                    tt('dve', pr4, oh, i4[:, :, which, :].unsqueeze(2).to_broadcast([128, 8, 16, 16]), ALU.mult, ['s2', 'idxf'], ['s_sb'])
                    red(selt[:].rearrange("p h s -> p (h s)"), s_sb[:].rearrange("p (x b) -> p x b", b=16), ['s_sb'], [selk])
                stt('dve', sel1[:], sel1[:], 128.0, sel2[:], ALU.mult, ALU.add, ['sel1', 'sel2'], ['sel1'])
                yield
                ts('dve', sel1[:], sel1[:], 0.0, float(NE - 1), ALU.max, ALU.min, ['sel1'], ['sel1'])
                cp('dve', idx_is[fb][:], sel1[:].rearrange("p h s -> p (h s)"), ['sel1'], ['idx_i%d' % fb])
                tt('dve', gatess[fb][:], tops[:], tops[:, :, 0:1].to_broadcast([128, 8, 16]), ALU.subtract, ['tops'], ['gates%d' % fb])
                act(gatess[fb][:], gatess[fb][:], AF.Exp, ['gates%d' % fb], ['gates%d' % fb])
                red(g8[:], gatess[fb][:], ['gates%d' % fb], ['g8'])
                op('dve', lambda e: e.reciprocal(out=g8[:], in_=g8[:]), reads=['g8'], writes=['g8'])
                tt('dve', gatess[fb][:], gatess[fb][:], g8[:].unsqueeze(2).to_broadcast([128, 8, 16]), ALU.mult, ['gates%d' % fb, 'g8'], ['gates%d' % fb])
                if li == 0:
                    dump('idx_0', sel1[:].rearrange("p h s -> p (h s)"), 'sel1')
                    dump('gates_0', gatess[fb][:].rearrange("p h s -> p (h s)"), 'gates%d' % fb)
                yield
            g0 = front(0)
            for _ in g0:
                pass
            for li in range(16):
                nxt = front(li + 1) if li + 1 < 16 else None
                g2d = gatess[li % 2][:].rearrange("p h s -> p (h s)")

                def vacc(g):
                    d_ = dg[g % 2]; dk = "dg%d" % (g % 2)
                    tt('dve', d_[:], identb[:].unsqueeze(1).to_broadcast([128, GS, 128]),
                       Aw[:, g * GS:(g + 1) * GS].unsqueeze(2).to_broadcast([128, GS, 128]), ALU.mult,
                       ['identb', 'Aw%d' % (g % 3)], [dk])
                    for q in range(GS):
                        s_ = g * GS + q
                        bk = "UV%d" % (s_ % NGB)
                        for hf in range(2):
                            pb, pk = bank(6 + hf)
                            mm(pb, d_[:, q, :], UV[s_ % NGB][:, D + hf * 512:D + (hf + 1) * 512], [dk, bk], [pk],
                               start=(s_ == 0), stop=(s_ == 127))
                ngrp = 128 // GS
                for g in range(ngrp):
                    gsl = slice(g * GS, (g + 1) * GS)
                    for q in range(GS):
                        s_ = g * GS + q
                        b_uv = UV[s_ % NGB]; bk = "UV%d" % (s_ % NGB)
                        dma('pool', b_uv[:], uv16_scr, reads=['idx_i%d' % (li % 2)], writes=[bk],
                            indirect=bass.IndirectOffsetOnAxis(ap=idx_is[li % 2][:, s_:s_ + 1], axis=0))
                        pr = prods[s_ % 2]; prk = "prod%d" % (s_ % 2)
                        tt('dve', pr[:], b_uv[:, 0:D], a2s[li % 2][:], ALU.mult, [bk, 'a2_%d' % (li % 2)], [prk])
                        act(junk[:], pr[:], AF.Identity, [prk], ['junk6', 'zsc%d' % (g % 3)], accum_out=zsc[:, s_:s_ + 1])
                    if g > 0:
                        vacc(g - 1)
                    act(Aw[:, gsl], zsc[:, gsl], AF.Gelu, ['zsc%d' % (g % 3)], ['Aw%d' % (g % 3)])
                    tt('dve', Aw[:, gsl], Aw[:, gsl], g2d[:, gsl], ALU.mult, ['Aw%d' % (g % 3), 'gates%d' % (li % 2)], ['Aw%d' % (g % 3)])
                    if nxt is not None:
                        for _ in range(1):
                            next(nxt, None)
                vacc(ngrp - 1)
                cp('dve', acc[:, 0:512], bank(6)[0], ['ps6'], ['acc'])
                cp('dve', acc[:, 512:1024], bank(7)[0], ['ps7'], ['acc'])
                if li == 0:
                    dump('f_0', acc[:], 'acc')
                if nxt is not None:
                    for _ in nxt:
                        pass
                tt('dve', acc[:], acc[:], bc2['m5'][:], ALU.mult, ['acc', 'b2_m5'], ['acc'])
                tt('dve', acc[:], acc[:], h1s[li % 2][:], ALU.add, ['acc', 'h1_%d' % (li % 2)], ['acc'])
                act(junk[:], acc[:], AF.Square, ['acc'], ['junk6', 'ss6b'], accum_out=ss6[:, 1:2])
                rstd_from(ss6[:, 1:2], D, 1e-6, 'ss6b')
                ts('dve', acc[:], acc[:], ss6[:, 1:2], None, ALU.mult, None, ['acc', 'ss6b'], ['acc'])
                tt('dve', acc[:], acc[:], bc2['fnw'][:], ALU.mult, ['acc', 'b2_fnw'], ['acc'])
                dma('sp', out_d[tokrows(li), :], acc[:], reads=['acc'])
        S.barrier()
    return nc


_CACHE = {}


def _consts():
    p = np.arange(128)[:, None]; f = np.arange(128)[None, :]
    le = (p <= f).astype(np.float32); lt = (p < f).astype(np.float32)
    ge = (p >= f).astype(np.float32); gt = (p > f).astype(np.float32)
    shm = np.zeros((128, 16), np.float32)
    pp = np.arange(128)
    shm[:, 0] = (pp % 64 != 0); shm[:, 1] = (pp % 64 != 63); shm[:, 2] = (pp >= 64); shm[:, 3] = (pp < 64)
    shm[:, 4] = 1.0; shm[:, 5] = (pp != 0); shm[:, 6] = (pp != 127); shm[:, 7] = -C0; shm[:, 8] = -C0
    iota = np.tile(np.arange(16, dtype=np.float32)[None, :], (128, 1))
    return np.concatenate([np.eye(128, dtype=np.float32), le, lt, ge, gt, le * (-1.0 / 16), ge * (-1.0 / 16),
                           le * (-C0), ge * (-C0), shm, iota], axis=1).astype(np.float32)


def make_in_maps(inp, cores):
    f = lambda a: np.ascontiguousarray(np.asarray(a, dtype=np.float32))
    wa = np.zeros((33, 1024), np.float32)
    wa[0:16, 0:512] = inp['gla_w_a2'][0, 0]; wa[16:32, 512:1024] = inp['gla_w_a2'][0, 1]
    wa[32, 0:512] = inp['gla_b_a'][0, 0]; wa[32, 512:1024] = inp['gla_b_a'][0, 1]
    w2a = np.concatenate([inp['rwkv_w2'][0], inp['rwkv_w0'][0][:, None, :]], axis=1)
    a2a = np.concatenate([inp['rwkv_a2'][0], inp['rwkv_a0'][0][None, :]], axis=0)
    shared = dict(
        norm1_w=f(inp['norm1_w'][0]), w_mod=f(inp['w_mod'][0]), b_mod=f(inp['b_mod'][0]), w_in=f(inp['w_in'][0]),
        gla_wa=f(wa), gla_norm_w=f(inp['gla_norm_w'][0]), rwkv_mu=f(inp['rwkv_mu'][0]), rwkv_w2a=f(w2a), rwkv_a2a=f(a2a),
        rwkv_g2=f(inp['rwkv_g2'][0]), rwkv_k_k=f(inp['rwkv_k_k'][0]), rwkv_k_a=f(inp['rwkv_k_a'][0]),
        rwkv_r_k=f(inp['rwkv_r_k'][0].reshape(-1)), rwkv_ln_w=f(inp['rwkv_ln_w'][0]), rwkv_ln_b=f(inp['rwkv_ln_b'][0]),
        w_out=f(inp['w_out'][0]), norm2_w=f(inp['norm2_w'][0]), peer_w_q=f(inp['peer_w_q'][0]),
        peer_sub_keys=f(inp['peer_sub_keys'][0].reshape(16, 128, 128)), peer_u=f(inp['peer_u'][0]), peer_v=f(inp['peer_v'][0]),
        final_norm_w=f(inp['final_norm_w']), cst=_consts())
    maps = []
    for b in cores:
        m = dict(shared)
        m['x'] = f(inp['x'][b]); m['ctx'] = f(inp['ctx'][b])
        m['c2'] = f(np.stack([inp['c'][b], inp['c_ctx']], axis=0))
        maps.append(m)
    return maps


def kernel(**inputs):
    if 'nc' not in _CACHE:
        _CACHE['nc'] = build_nc()
    nc = _CACHE['nc']
    maps = make_in_maps(inputs, list(range(8)))
    res = run_bass_kernel_spmd(nc, maps, core_ids=list(range(8)))
    return np.stack([np.asarray(r['out'], dtype=np.float32) for r in res.results], axis=0)
```
